# Optimizing a Trainium2 kernel written in Bass

```python
import math
import jax
import jax.numpy as jnp
from jax import lax
import numpy as np

D_MODEL = 1024
BATCH = 4
SEQ = 8192
DEPTH = 2

HEAD_DIM = 64
GDN_WIDTH = 3 * D_MODEL // 8
CONV_WIDTH = D_MODEL // 4
FOX_WIDTH = D_MODEL - GDN_WIDTH - CONV_WIDTH
MIX_WIDTH = GDN_WIDTH + CONV_WIDTH + FOX_WIDTH
GDN_HEADS = GDN_WIDTH // HEAD_DIM
FOX_HEADS = FOX_WIDTH // HEAD_DIM
GDN_SHORT_CONV = 4
GDN_CHUNK = 64
CONV_KERNEL = 31
FOX_BLOCK = 128
FFN_DENSE = ((8 * D_MODEL // 3 + 127) // 128) * 128
N_EXPERTS = 8
TOP_K = 2
FFN_EXPERT = 7 * D_MODEL // 2
MOE_BLOCK = 128
N_DENSE = (DEPTH + 1) // 2
N_MOE = DEPTH // 2
DEEPNORM_ALPHA = (2 * DEPTH) ** 0.25
DEEPNORM_BETA = (8 * DEPTH) ** -0.25
LN_EPS = 1e-5
NORM_EPS = 1e-6
IN_SIZES = (GDN_WIDTH, GDN_WIDTH, GDN_WIDTH, GDN_HEADS, GDN_HEADS, GDN_WIDTH,
            2 * CONV_WIDTH,
            FOX_WIDTH, FOX_WIDTH, FOX_WIDTH, FOX_HEADS)
IN_COLS = sum(IN_SIZES)

kernel_name = 'hybrid_gdn_conformer_fox_moe_deepnorm'


def layer_norm(x, g, b):
    xf = x.astype(jnp.float32)
    mu = xf.mean(-1, keepdims=True)
    var = jnp.square(xf - mu).mean(-1, keepdims=True)
    return ((xf - mu) * lax.rsqrt(var + LN_EPS) * g.astype(jnp.float32) + b.astype(jnp.float32)).astype(x.dtype)


def rms_norm_f32(x, g):
    xf = x.astype(jnp.float32)
    return xf * lax.rsqrt(jnp.square(xf).mean(-1, keepdims=True) + NORM_EPS) * g.astype(jnp.float32)


def l2_normalize(x):
    return x * lax.rsqrt(jnp.sum(jnp.square(x), -1, keepdims=True) + NORM_EPS)


def split_columns(p, sizes):
    cuts = np.cumsum(np.array(sizes))[:-1].tolist()
    return jnp.split(p, cuts, axis=-1)


def causal_depthwise_conv(x, w):
    k_width, channels = w.shape
    return lax.conv_general_dilated(
        x, w[:, None, :], window_strides=(1,), padding=[(k_width - 1, 0)],
        dimension_numbers=('NWC', 'WIO', 'NWC'), feature_group_count=channels)


def gated_delta_rule(q, k, v, g, beta):
    B, T, H, dk = q.shape
    dv = v.shape[-1]
    C = GDN_CHUNK
    n = T // C
    f32 = jnp.float32
    q = l2_normalize(q.astype(f32)) * dk ** -0.5
    k = l2_normalize(k.astype(f32))
    v = v.astype(f32)

    def chunks(t):
        return t.reshape(B, n, C, H, -1).transpose(1, 0, 3, 2, 4)

    qc, kc, vc = chunks(q), chunks(k), chunks(v)
    gc = jnp.cumsum(chunks(g.astype(f32)[..., None])[..., 0], axis=-1)
    bc = chunks(beta.astype(f32)[..., None])
    causal = jnp.tril(jnp.ones((C, C), bool))
    strict = jnp.tril(jnp.ones((C, C), bool), -1)
    diff = gc[..., :, None] - gc[..., None, :]
    decay = jnp.where(causal, jnp.exp(jnp.where(causal, diff, 0.0)), 0.0)
    kb = kc * bc
    lower = jnp.where(strict, jnp.einsum('nbhcd,nbhsd->nbhcs', kb, kc) * decay, 0.0)
    eye = jnp.eye(C, dtype=f32)
    t_inv = lax.linalg.triangular_solve(lower + eye, jnp.broadcast_to(eye, lower.shape),
                                        left_side=True, lower=True, unit_diagonal=True)
    u = jnp.einsum('nbhcs,nbhsd->nbhcd', t_inv, vc * bc)
    w = jnp.einsum('nbhcs,nbhsd->nbhcd', t_inv, kb * jnp.exp(gc)[..., None])
    a_intra = jnp.where(causal, jnp.einsum('nbhcd,nbhsd->nbhcs', qc, kc) * decay, 0.0)

    def step(state, inp):
        q_i, k_i, u_i, w_i, a_i, g_i = inp
        v_new = u_i - jnp.einsum('bhck,bhkv->bhcv', w_i, state)
        o_i = (jnp.einsum('bhck,bhkv->bhcv', q_i * jnp.exp(g_i)[..., None], state)
               + jnp.einsum('bhcs,bhsv->bhcv', a_i, v_new))
        g_last = g_i[..., -1]
        state = (state * jnp.exp(g_last)[..., None, None]
                 + jnp.einsum('bhck,bhcv->bhkv', k_i * jnp.exp(g_last[..., None] - g_i)[..., None], v_new))
        return state, o_i

    s0 = jnp.zeros((B, H, dk, dv), f32)
    _, o = lax.scan(step, s0, (qc, kc, u, w, a_intra, gc))
    return o.transpose(1, 0, 3, 2, 4).reshape(B, T, H, dv)


def forgetting_attention(q, k, v, log_f):
    B, T, H, d = q.shape
    f32 = jnp.float32
    nb = T // FOX_BLOCK
    c = jnp.cumsum(log_f.astype(f32), axis=1)
    qb = (q.astype(f32) * d ** -0.5).reshape(B, nb, FOX_BLOCK, H, d).transpose(1, 0, 3, 2, 4)
    cq = c.reshape(B, nb, FOX_BLOCK, H).transpose(1, 0, 3, 2)
    kt = k.astype(f32).transpose(0, 2, 1, 3)
    vt = v.astype(f32).transpose(0, 2, 1, 3)
    ck = c.transpose(0, 2, 1)
    key_pos = jnp.arange(T)

    def block(args):
        q_blk, cq_blk, i = args
        q_pos = i * FOX_BLOCK + jnp.arange(FOX_BLOCK)
        s = jnp.einsum('bhqd,bhkd->bhqk', q_blk, kt) + cq_blk[..., None] - ck[:, :, None, :]
        s = jnp.where(key_pos[None, :] <= q_pos[:, None], s, -jnp.inf)
        p = jax.nn.softmax(s, axis=-1)
        return jnp.einsum('bhqk,bhkd->bhqd', p, vt)

    o = lax.map(block, (qb, cq, jnp.arange(nb)))
    return o.transpose(1, 0, 3, 2, 4).reshape(B, T, H, d)


def hybrid_mixer(x, w_in, mix_scale, w_out, gdn_conv_w, gdn_a_log, gdn_dt_bias, gdn_norm_w,
                 cnv_dw_w, cnv_dw_b, cnv_ln_g, cnv_ln_b, fox_f_bias):
    B, T, _ = x.shape
    dt = x.dtype
    f32 = jnp.float32
    proj = jnp.einsum('btd,de->bte', x, w_in)
    (a_q, a_k, a_v, a_decay, a_beta, a_gate, c_glu, f_q, f_k, f_v, f_forget) = split_columns(proj, IN_SIZES)

    def heads(t, h):
        return t.reshape(B, T, h, HEAD_DIM)

    qkv = jax.nn.silu(causal_depthwise_conv(jnp.concatenate([a_q, a_k, a_v], -1), gdn_conv_w))
    a_q, a_k, a_v = jnp.split(qkv, 3, axis=-1)
    log_decay = -jnp.exp(gdn_a_log.astype(f32)) * jax.nn.softplus(a_decay.astype(f32) + gdn_dt_bias.astype(f32))
    beta = jax.nn.sigmoid(a_beta.astype(f32))
    o_a = gated_delta_rule(heads(a_q, GDN_HEADS), heads(a_k, GDN_HEADS), heads(a_v, GDN_HEADS), log_decay, beta)
    o_a = (rms_norm_f32(o_a, gdn_norm_w) * jax.nn.silu(heads(a_gate, GDN_HEADS).astype(f32))).reshape(B, T, GDN_WIDTH).astype(dt)

    glu_a, glu_b = jnp.split(c_glu, 2, axis=-1)
    u = glu_a * jax.nn.sigmoid(glu_b)
    u = causal_depthwise_conv(u, cnv_dw_w) + cnv_dw_b
    o_b = jax.nn.silu(layer_norm(u, cnv_ln_g, cnv_ln_b))

    log_f = jax.nn.log_sigmoid(f_forget.astype(f32) + fox_f_bias.astype(f32))
    o_c = forgetting_attention(heads(f_q, FOX_HEADS), heads(f_k, FOX_HEADS), heads(f_v, FOX_HEADS), log_f)
    o_c = o_c.reshape(B, T, FOX_WIDTH).astype(dt)

    mixed = jnp.concatenate([o_a, o_b, o_c], axis=-1) * mix_scale
    return jnp.einsum('bte,ed->btd', mixed, w_out)


def swiglu(x, w1, w3, w2):
    h = jax.nn.silu(jnp.einsum('btd,df->btf', x, w1)) * jnp.einsum('btd,df->btf', x, w3)
    return jnp.einsum('btf,fd->btd', h, w2)


def moe_swiglu(x, w_router, w1, w3, w2):
    B, T, D = x.shape
    xf = x.reshape(-1, D)
    n_tok = xf.shape[0]
    n_asg = n_tok * TOP_K
    logits = jnp.einsum('nd,de->ne', xf, w_router).astype(jnp.float32)
    top_logits, top_idx = lax.top_k(logits, TOP_K)
    gates = jax.nn.softmax(top_logits, axis=-1)
    e_flat = top_idx.reshape(-1)
    order = jnp.argsort(e_flat)
    e_sorted = e_flat[order]
    sizes = jnp.bincount(e_flat, length=N_EXPERTS).astype(jnp.int32)
    padded = ((sizes + MOE_BLOCK - 1) // MOE_BLOCK) * MOE_BLOCK
    pad_end = jnp.cumsum(padded)
    pad_start = pad_end - padded
    start = jnp.cumsum(sizes) - sizes
    rank = jnp.arange(n_asg, dtype=jnp.int32) - start[e_sorted]
    dest = pad_start[e_sorted] + rank
    n_blocks = (n_asg + MOE_BLOCK - 1) // MOE_BLOCK + N_EXPERTS
    rows = n_blocks * MOE_BLOCK
    tok_pad = jnp.full((rows,), n_tok, jnp.int32).at[dest].set((order // TOP_K).astype(jnp.int32))
    gate_pad = jnp.zeros((rows,), jnp.float32).at[dest].set(gates.reshape(-1)[order])
    block_start = jnp.arange(n_blocks, dtype=jnp.int32) * MOE_BLOCK
    block_expert = jnp.minimum(jnp.searchsorted(pad_end, block_start, side='right'), N_EXPERTS - 1)
    x_ext = jnp.concatenate([xf, jnp.zeros((1, D), xf.dtype)], axis=0)
    x_blocks = x_ext[tok_pad].reshape(n_blocks, MOE_BLOCK, D)

    def expert_block(args):
        xb, e = args
        h = jax.nn.silu(xb @ w1[e]) * (xb @ w3[e])
        return h @ w2[e]

    y = lax.map(expert_block, (x_blocks, block_expert)).reshape(rows, D)
    y = y * gate_pad[:, None].astype(y.dtype)
    out = jnp.zeros((n_tok + 1, D), y.dtype).at[tok_pad].add(y)[:n_tok]
    return out.reshape(B, T, D)


def setup_inputs(seed: int = 0) -> dict:
    key = jax.random.key(seed)
    ks = jax.random.split(key, 26)
    f32 = jnp.float32
    nrm = jax.random.normal
    x = nrm(ks[0], (BATCH, SEQ, D_MODEL), f32)
    w_in = nrm(ks[1], (DEPTH, D_MODEL, IN_COLS), f32) * D_MODEL ** -0.5
    mix_scale = 1.0 + 0.05 * nrm(ks[2], (DEPTH, MIX_WIDTH), f32)
    w_out = nrm(ks[3], (DEPTH, MIX_WIDTH, D_MODEL), f32) * (MIX_WIDTH ** -0.5 * DEEPNORM_BETA)
    gdn_conv_w = nrm(ks[4], (DEPTH, GDN_SHORT_CONV, 3 * GDN_WIDTH), f32) * GDN_SHORT_CONV ** -0.5
    gdn_a_log = jnp.log(jax.random.uniform(ks[5], (DEPTH, GDN_HEADS), f32, 1.0, 16.0))
    dt_init = jnp.exp(jax.random.uniform(ks[6], (DEPTH, GDN_HEADS), f32, math.log(1e-3), math.log(1e-1)))
    gdn_dt_bias = dt_init + jnp.log(-jnp.expm1(-dt_init))
    gdn_norm_w = 1.0 + 0.05 * nrm(ks[7], (DEPTH, HEAD_DIM), f32)
    cnv_dw_w = nrm(ks[8], (DEPTH, CONV_KERNEL, CONV_WIDTH), f32) * CONV_KERNEL ** -0.5
    cnv_dw_b = 0.02 * nrm(ks[9], (DEPTH, CONV_WIDTH), f32)
    cnv_ln_g = 1.0 + 0.05 * nrm(ks[10], (DEPTH, CONV_WIDTH), f32)
    cnv_ln_b = 0.02 * nrm(ks[11], (DEPTH, CONV_WIDTH), f32)
    fox_f_bias = 3.0 + 0.5 * nrm(ks[12], (DEPTH, FOX_HEADS), f32)
    ln_mix_g = 1.0 + 0.05 * nrm(ks[13], (DEPTH, D_MODEL), f32)
    ln_mix_b = 0.02 * nrm(ks[14], (DEPTH, D_MODEL), f32)
    ln_ffn_g = 1.0 + 0.05 * nrm(ks[15], (DEPTH, D_MODEL), f32)
    ln_ffn_b = 0.02 * nrm(ks[16], (DEPTH, D_MODEL), f32)
    ffn_w1 = nrm(ks[17], (N_DENSE, D_MODEL, FFN_DENSE), f32) * D_MODEL ** -0.5
    ffn_w3 = nrm(ks[18], (N_DENSE, D_MODEL, FFN_DENSE), f32) * D_MODEL ** -0.5
    ffn_w2 = nrm(ks[19], (N_DENSE, FFN_DENSE, D_MODEL), f32) * (FFN_DENSE ** -0.5 * DEEPNORM_BETA)
    moe_router = nrm(ks[20], (N_MOE, D_MODEL, N_EXPERTS), f32) * D_MODEL ** -0.5
    moe_w1 = nrm(ks[21], (N_MOE, N_EXPERTS, D_MODEL, FFN_EXPERT), f32) * D_MODEL ** -0.5
    moe_w3 = nrm(ks[22], (N_MOE, N_EXPERTS, D_MODEL, FFN_EXPERT), f32) * D_MODEL ** -0.5
    moe_w2 = nrm(ks[23], (N_MOE, N_EXPERTS, FFN_EXPERT, D_MODEL), f32) * (FFN_EXPERT ** -0.5 * DEEPNORM_BETA)
    return {'x': x, 'w_in': w_in, 'mix_scale': mix_scale, 'w_out': w_out,
            'gdn_conv_w': gdn_conv_w, 'gdn_a_log': gdn_a_log, 'gdn_dt_bias': gdn_dt_bias, 'gdn_norm_w': gdn_norm_w,
            'cnv_dw_w': cnv_dw_w, 'cnv_dw_b': cnv_dw_b, 'cnv_ln_g': cnv_ln_g, 'cnv_ln_b': cnv_ln_b,
            'fox_f_bias': fox_f_bias,
            'ln_mix_g': ln_mix_g, 'ln_mix_b': ln_mix_b, 'ln_ffn_g': ln_ffn_g, 'ln_ffn_b': ln_ffn_b,
            'ffn_w1': ffn_w1, 'ffn_w3': ffn_w3, 'ffn_w2': ffn_w2,
            'moe_router': moe_router, 'moe_w1': moe_w1, 'moe_w3': moe_w3, 'moe_w2': moe_w2}


def reference(x, w_in, mix_scale, w_out, gdn_conv_w, gdn_a_log, gdn_dt_bias, gdn_norm_w,
              cnv_dw_w, cnv_dw_b, cnv_ln_g, cnv_ln_b, fox_f_bias,
              ln_mix_g, ln_mix_b, ln_ffn_g, ln_ffn_b,
              ffn_w1, ffn_w3, ffn_w2, moe_router, moe_w1, moe_w3, moe_w2):
    for l in range(DEPTH):
        mix = hybrid_mixer(x, w_in[l], mix_scale[l], w_out[l], gdn_conv_w[l], gdn_a_log[l], gdn_dt_bias[l],
                           gdn_norm_w[l], cnv_dw_w[l], cnv_dw_b[l], cnv_ln_g[l], cnv_ln_b[l], fox_f_bias[l])
        x = layer_norm(DEEPNORM_ALPHA * x + mix, ln_mix_g[l], ln_mix_b[l])
        if l % 2 == 0:
            ff = swiglu(x, ffn_w1[l // 2], ffn_w3[l // 2], ffn_w2[l // 2])
        else:
            ff = moe_swiglu(x, moe_router[l // 2], moe_w1[l // 2], moe_w3[l // 2], moe_w2[l // 2])
        x = layer_norm(DEEPNORM_ALPHA * x + ff, ln_ffn_g[l], ln_ffn_b[l])
    return x
```

```python
import contextlib
import math
import numpy as np
import ml_dtypes
import concourse.bass as bass
import concourse.mybir as mybir
from concourse.bass_utils import run_bass_kernel_spmd

F32 = mybir.dt.float32
BF16 = mybir.dt.bfloat16
ALU = mybir.AluOpType
AF = mybir.ActivationFunctionType
AX = mybir.AxisListType

ENGS = ["tensor", "vector", "scalar", "gpsimd", "sync"]

D = 1024
HD = 64
NGH = 3
NFH = 3
CONVW = 256
CK = 31
FFN_DENSE = 2816
NEXP = 8
FFN_EXP = 3584
ALPHA = 4 ** 0.25
LN_EPS = 1e-5
NORM_EPS = 1e-6
OFF = dict(a_q=0, a_k=384, a_v=768, a_decay=1152, a_beta=1158, a_gate=1164, glu_a=1548, glu_b=1804,
           f_q=2060, f_k=2444, f_v=2828, f_forget=3212)
BIG = 30000.0
DBG = set()
DBGV = {}
DQ2 = "gpsimd"


class Res:
    __slots__ = ("name", "last_w", "readers", "excl")

    def __init__(self, name, excl=False):
        self.name = name
        self.last_w = None
        self.readers = []
        self.excl = excl


class Op:
    __slots__ = ("eng", "fn", "deps", "is_dma", "signal", "count", "dma_sem", "dma_count", "is_cc")

    def __init__(self, eng, fn, is_dma, is_cc=False):
        self.eng = eng
        self.fn = fn
        self.deps = set()
        self.is_dma = is_dma or is_cc
        self.is_cc = is_cc
        self.signal = False
        self.count = 0
        self.dma_sem = None
        self.dma_count = 0


class Sched:
    NL = 8
    _uid = 0

    def __init__(self, nc):
        self.nc = nc
        self.ops = []

    def op(self, eng, fn, reads=(), writes=(), dma=False, cc=False):
        o = Op(eng, fn, dma, cc)
        idx = len(self.ops)
        writes = [r for r in writes if r is not None] + [r for r in reads if r is not None and r.excl]
        reads = [r for r in reads if r is not None and not r.excl]
        for r in reads:
            if r is not None and r.last_w is not None:
                o.deps.add(r.last_w)
        for r in writes:
            if r is None:
                continue
            if r.last_w is not None:
                o.deps.add(r.last_w)
            for rd in r.readers:
                o.deps.add(rd)
        for r in reads:
            if r is not None:
                r.readers.append(idx)
        for r in writes:
            if r is not None:
                r.last_w = idx
                r.readers = []
        o.deps.discard(idx)
        self.ops.append(o)
        return idx

    def emit(self):
        nc = self.nc
        ops = self.ops
        NL = self.NL
        for o in ops:
            if o.eng == "tensor" and not o.is_dma:
                o.deps = {d for d in o.deps if not (ops[d].eng == "tensor" and not ops[d].is_dma)}
        dn0 = {e: 0 for e in ENGS}
        for o in ops:
            if o.is_cc:
                o.dma_sem = ("cc", 0)
            elif o.is_dma:
                o.dma_sem = (o.eng, dn0[o.eng] % NL)
                dn0[o.eng] += 1
        for o in ops:
            best = {}
            for d in o.deps:
                od = ops[d]
                key = od.dma_sem if od.is_dma else od.eng
                if d > best.get(key, -1):
                    best[key] = d
            o.deps = set(best.values())
        for o in ops:
            for d in o.deps:
                ops[d].signal = True
        cnt = {e: 0 for e in ENGS}
        dn = {e: 0 for e in ENGS}
        lane_cnt = {}
        for o in ops:
            if o.is_cc:
                lane = ("cc", 0)
                lane_cnt[lane] = lane_cnt.get(lane, 0) + 1
                o.dma_sem = lane
                o.dma_count = lane_cnt[lane]
            elif o.is_dma:
                lane = (o.eng, dn[o.eng] % NL)
                dn[o.eng] += 1
                lane_cnt[lane] = lane_cnt.get(lane, 0) + 16
                o.dma_sem = lane
                o.dma_count = lane_cnt[lane]
            elif o.signal:
                cnt[o.eng] += 1
                o.count = cnt[o.eng]
        Sched._uid += 1
        u = Sched._uid
        sem = {e: nc.alloc_semaphore(name=f"s{u}_{e}") for e in ENGS}
        dsem = {}
        for lane in lane_cnt:
            dsem[lane] = nc.alloc_semaphore(name=f"d{u}_{lane[0]}_{lane[1]}")
        with contextlib.ExitStack() as st:
            block = st.enter_context(nc.Block())
            per_eng = {e: [] for e in ENGS}
            for i, o in enumerate(ops):
                per_eng[o.eng].append(i)

            def body_for(e):
                def body(engine):
                    waited = {}
                    for i in per_eng[e]:
                        o = ops[i]
                        need = {}
                        for d in o.deps:
                            od = ops[d]
                            key = ("d", od.dma_sem) if od.is_dma else ("c", od.eng)
                            v = od.dma_count if od.is_dma else od.count
                            if v > need.get(key, 0):
                                need[key] = v
                        if o.is_dma and not o.is_cc and o.dma_count > 16:
                            key = ("d", o.dma_sem)
                            need[key] = max(need.get(key, 0), o.dma_count - 16)
                        for key, v in need.items():
                            if waited.get(key, 0) >= v:
                                continue
                            s = dsem[key[1]] if key[0] == "d" else sem[key[1]]
                            engine.wait_ge(s, v)
                            waited[key] = v
                        ins = o.fn(engine)
                        if o.is_cc:
                            ins.then_inc(dsem[o.dma_sem])
                        elif o.is_dma:
                            ins.then_inc(dsem[o.dma_sem], 16)
                        elif o.signal:
                            ins.then_inc(sem[e], 1)
                    for lane, v in lane_cnt.items():
                        if lane[0] == e or (lane[0] == "cc" and e == "gpsimd"):
                            engine.wait_ge(dsem[lane], v)
                return body

            for e in ENGS:
                if per_eng[e]:
                    getattr(block, e)(body_for(e))
        nc.clear_and_free_semaphores(list(sem.values()) + list(dsem.values()))
        nc.all_engine_barrier()
        return cnt, dn


class V:
    __slots__ = ("ap", "r")

    def __init__(self, ap, r):
        self.ap = ap
        self.r = r

    def __getitem__(self, idx):
        return V(self.ap[idx], self.r)


class Tl:
    def __init__(self, t, r):
        self.t = t
        self.r = r

    def __getitem__(self, idx):
        return V(self.t[idx], self.r)


def _aps(x):
    return x.ap if isinstance(x, V) else x


def _rs(*xs):
    return [x.r for x in xs if isinstance(x, V) and x.r is not None]


class Phase:
    _uid = [0]

    def __init__(self, nc, name):
        self.nc = nc
        Phase._uid[0] += 1
        self.name = f"{name}{Phase._uid[0]}"

    def __enter__(self):
        self.st = contextlib.ExitStack()
        self.S = Sched(self.nc)
        self.n = 0
        return self

    def __exit__(self, *exc):
        if exc[0] is None:
            self.S.emit()
        self.st.close()
        return False

    def tile(self, shape, dtype, name=None):
        self.n += 1
        nm = f"{self.name}_{name or 't'}{self.n}"
        t = self.st.enter_context(self.nc.sbuf_tensor(nm, list(shape), dtype))
        return Tl(t, Res(nm))

    def psum(self, shape, dtype=F32, name=None):
        self.n += 1
        nm = f"{self.name}_{name or 'ps'}{self.n}"
        nb = 2 if dtype == BF16 else 4
        assert int(np.prod(shape[1:])) * nb == 2048 and shape[0] == 128, "PSUM tiles are whole banks"
        t = self.st.enter_context(self.nc.psum_tensor(nm, list(shape), dtype))
        return Tl(t, Res(nm, excl=True))

    def dma(self, q, out, in_, **kw):
        o, i = _aps(out), _aps(in_)
        return self.S.op(q, lambda e: e.dma_start(out=o, in_=i, **kw), reads=_rs(in_), writes=_rs(out), dma=True)

    def mm(self, out, lhsT, rhs, start=True, stop=True, extra_reads=()):
        o, l, r = _aps(out), _aps(lhsT), _aps(rhs)
        return self.S.op("tensor", lambda e: e.matmul(o, lhsT=l, rhs=r, start=start, stop=stop),
                         reads=_rs(lhsT, rhs) + list(extra_reads), writes=_rs(out))

    def tr(self, out, in_, ident):
        o, i, d = _aps(out), _aps(in_), _aps(ident)
        return self.S.op("tensor", lambda e: e.transpose(o, i, d), reads=_rs(in_, ident), writes=_rs(out))

    def act(self, out, in_, func, bias=None, scale=None, accum=None, eng="scalar"):
        o, i = _aps(out), _aps(in_)
        kw = {}
        if bias is not None:
            kw["bias"] = _aps(bias)
        if scale is not None:
            kw["scale"] = _aps(scale)
        if accum is not None:
            kw["accum_out"] = _aps(accum)
        return self.S.op("scalar", lambda e: e.activation(out=o, in_=i, func=func, **kw),
                         reads=_rs(in_, bias, scale), writes=_rs(out, accum))

    def ts(self, eng, out, in0, s1, s2, op0, op1=None, accum=None):
        o, i, a, b = _aps(out), _aps(in0), _aps(s1), _aps(s2)
        kw = {}
        if op1 is not None:
            kw["op1"] = op1
        if accum is not None:
            kw["accum_out"] = _aps(accum)
        return self.S.op(eng, lambda e: e.tensor_scalar(out=o, in0=i, scalar1=a, scalar2=b, op0=op0, **kw),
                         reads=_rs(in0, s1, s2), writes=_rs(out, accum))

    def tt(self, eng, out, in0, in1, op):
        o, i, j = _aps(out), _aps(in0), _aps(in1)
        return self.S.op(eng, lambda e: e.tensor_tensor(out=o, in0=i, in1=j, op=op),
                         reads=_rs(in0, in1), writes=_rs(out))

    def stt(self, eng, out, in0, scalar, in1, op0, op1, accum=None):
        o, i, s, j = _aps(out), _aps(in0), _aps(scalar), _aps(in1)
        kw = {}
        if accum is not None:
            kw["accum_out"] = _aps(accum)
        return self.S.op(eng, lambda e: e.scalar_tensor_tensor(out=o, in0=i, scalar=s, in1=j, op0=op0, op1=op1, **kw),
                         reads=_rs(in0, scalar, in1), writes=_rs(out, accum))

    def copy(self, eng, out, in_):
        o, i = _aps(out), _aps(in_)
        if eng == "scalar":
            return self.S.op(eng, lambda e: e.copy(out=o, in_=i), reads=_rs(in_), writes=_rs(out))
        return self.S.op(eng, lambda e: e.tensor_copy(out=o, in_=i), reads=_rs(in_), writes=_rs(out))

    def recip(self, out, in_, eng="vector"):
        o, i = _aps(out), _aps(in_)
        return self.S.op(eng, lambda e: e.reciprocal(out=o, in_=i), reads=_rs(in_), writes=_rs(out))

    def memset(self, eng, out, val):
        o = _aps(out)
        return self.S.op(eng, lambda e: e.memset(o, val), writes=_rs(out))

    def scan(self, out, d0, d1, init, op0, op1):
        o, a, b, c = _aps(out), _aps(d0), _aps(d1), _aps(init)
        return self.S.op("vector", lambda e: e.tensor_tensor_scan(out=o, data0=a, data1=b, initial=c, op0=op0, op1=op1),
                         reads=_rs(d0, d1, init), writes=_rs(out))


FM_GROUPS = [("g%s%d" % (t, h), 64) for h in range(3) for t in "qkv"] + [
             ("fq01", 128), ("fq2", 64), ("fk01", 128), ("fk2", 64),
             ("ca0", 128), ("cb0", 128), ("ca1", 128), ("cb1", 128)]
FM_OFF = {}
_o = 0
for _n, _m in FM_GROUPS:
    FM_OFF[_n] = (_o, _m)
    _o += _m
TM0 = _o
NTM = 9 + 192 + 192
NCOL = TM0 + NTM


def conv_perm(hh):
    return list(range(128 * hh, 128 * hh + 128)) + list(range(128 * (1 - hh), 128 * (1 - hh) + 128))


def pack_w_in(w_in_l, hh):
    def heads(base, h0, n):
        return list(range(base + (3 * hh + h0) * 64, base + (3 * hh + h0 + n) * 64))
    cols = []
    for h in range(3):
        cols += heads(OFF["a_q"], h, 1) + heads(OFF["a_k"], h, 1) + heads(OFF["a_v"], h, 1)
    cols += heads(OFF["f_q"], 0, 2) + heads(OFF["f_q"], 2, 1) + heads(OFF["f_k"], 0, 2) + heads(OFF["f_k"], 2, 1)
    perm = conv_perm(hh)
    cols += [OFF["glu_a"] + c for c in perm[:128]] + [OFF["glu_b"] + c for c in perm[:128]]
    cols += [OFF["glu_a"] + c for c in perm[128:]] + [OFF["glu_b"] + c for c in perm[128:]]
    cols += [OFF["a_decay"] + 3 * hh + i for i in range(3)] + [OFF["a_beta"] + 3 * hh + i for i in range(3)]
    cols += [OFF["f_forget"] + 3 * hh + i for i in range(3)]
    cols += heads(OFF["a_gate"], 0, 3) + heads(OFF["f_v"], 0, 3)
    assert len(cols) == NCOL
    return np.ascontiguousarray(w_in_l[:, cols])


def pack_gcw(gdn_conv_w_l, hh):
    out = np.zeros((128, 9, 4), np.float32)
    for h in range(3):
        for typ in range(3):
            ch0 = typ * 384 + (3 * hh + h) * 64
            out[:64, h * 3 + typ, :] = gdn_conv_w_l[:, ch0: ch0 + 64].T
    return out


def phase_A(nc, T, d, layer0):
    NB = T // 512
    with Phase(nc, "A") as P:
        ident = P.tile([128, 128], BF16, "ident")
        P.dma("sync", ident[:, :], d["ident_bf"])
        w = P.tile([128, 8, NCOL], BF16, "w")
        stage = [P.tile([128, NCOL], F32, "wst") for _ in range(2)]
        for k in range(8):
            s = stage[k % 2]
            P.dma("sync", s[:, :], d["w_in"][k * 128:(k + 1) * 128, :])
            P.copy(["vector", "gpsimd"][k % 2], w[:, k, :], s[:, :])
        gcw = P.tile([128, 9, 4], F32, "gcw")
        P.dma("sync", gcw[:, :, :], d["gcw"])
        bc9 = P.tile([128, 9], F32, "bc9")
        P.dma("sync", bc9[:, :], d["bc9"])
        dtb4 = P.tile([128, 4, 3], F32, "dtb4")
        negA4 = P.tile([128, 4, 3], F32, "negA4")
        fb4 = P.tile([128, 4, 3], F32, "fb4")
        eA = P.tile([128, 3], F32, "eA")
        P.act(eA[:, :], bc9[:, 3:6], AF.Exp)
        for tt in range(4):
            P.copy("vector", dtb4[:, tt, :], bc9[:, 0:3])
            P.ts("vector", negA4[:, tt, :], eA[:, :], -1.0, None, ALU.mult)
            P.copy("vector", fb4[:, tt, :], bc9[:, 6:9])

        fm_ps = [P.psum([128, 512], F32, "fm") for _ in range(4)]
        tm_ps = [P.psum([128, 512], F32, "tm") for _ in range(2)]
        xtb = [P.tile([128, 8, 512], BF16, "xt") for _ in range(2)]
        if layer0:
            tr_ps = [P.psum([128, 8, 128], BF16, "tr") for _ in range(2)]
            xst = [P.tile([128, D], F32, "xst") for _ in range(2)]
            xbf = [P.tile([128, D], BF16, "xbf") for _ in range(2)]
        raw = [P.tile([128, 515], F32, "raw") for _ in range(9)]
        for r in raw:
            P.memset("vector", r[:, 0:3], 0.0)
        acc = [P.tile([128, 512], F32, "acc") for _ in range(2)]
        ybf = [P.tile([128, 512], BF16, "ybf") for _ in range(4)]
        sig = [P.tile([128, 512], F32, "sig") for _ in range(2)]
        uu = [P.tile([128, 512], F32, "uu") for _ in range(2)]
        small = [P.tile([128, 4, 9], F32, "small") for _ in range(2)]
        sm_t = [P.tile([128, 4, 9], F32, "smt") for _ in range(2)]
        sm_o = [P.tile([128, 4, 9], F32, "smo") for _ in range(2)]
        gate_bf = [P.tile([128, 192], BF16, "gate") for _ in range(2)]
        fv_bf = [P.tile([128, 192], BF16, "fv") for _ in range(2)]
        zero = P.tile([128, 32], F32, "zero")
        P.memset("vector", zero[:, :], 0.0)
        for i in range(2):
            P.dma(DQ2, d["CU"][i * 128:(i + 1) * 128, 0:30], zero[:, 0:30])

        n_fm = 0
        n_y = 0
        n_tt = 0
        for blk in range(NB):
            t0 = blk * 512
            xt = xtb[blk % 2]
            if layer0:
                for tt in range(4):
                    j = blk * 4 + tt
                    xs, xb, ps = xst[j % 2], xbf[j % 2], tr_ps[j % 2]
                    P.dma("sync", xs[:, :], d["x"][t0 + tt * 128: t0 + (tt + 1) * 128, :])
                    P.copy("gpsimd", xb[:, :], xs[:, :])
                    for k in range(8):
                        P.tr(ps[:, k, :], xb[:, k * 128:(k + 1) * 128], ident[:, :])
                    P.copy("scalar", xt[:, :, tt * 128:(tt + 1) * 128], ps[:, :, :])
            else:
                P.dma("sync", xt[:, :, :], d["XT"][:, :, t0:t0 + 512].rearrange("k p t -> p k t"))

            glu_a_ps = None
            for gi, (gname, M) in enumerate(FM_GROUPS):
                if ("gdn" in DBG and gi < 9) or ("fox" in DBG and gname[0] == "f") or ("glu" in DBG and gname[0] == "c"):
                    continue
                c0 = FM_OFF[gname][0]
                ps = fm_ps[n_fm % 4]
                n_fm += 1
                for k in range(8):
                    P.mm(ps[0:M, :], w[:, k, c0:c0 + M], xt[:, k, :], start=(k == 0), stop=(k == 7))
                if gi < 9:
                    r = raw[gi]
                    P.copy("scalar", r[0:M, 3:515], ps[0:M, :])
                    a = acc[n_y % 2]
                    P.ts("vector", a[0:M, :], r[0:M, 0:512], gcw[0:M, gi, 0:1], None, ALU.mult)
                    for j in range(1, 4):
                        P.stt("vector", a[0:M, :], r[0:M, j:j + 512], gcw[0:M, gi, j:j + 1], a[0:M, :], ALU.mult, ALU.add)
                    y = ybf[n_y % 4]
                    n_y += 1
                    P.act(y[0:M, :], a[0:M, :], AF.Silu)
                    P.dma(DQ2, d["GQ"][gi, 0:M, t0:t0 + 512], y[0:M, :])
                    P.copy("gpsimd", r[0:M, 0:3], r[0:M, 512:515])
                elif gname in ("fq01", "fq2", "fk01", "fk2"):
                    y = ybf[n_y % 4]
                    n_y += 1
                    if gname[1] == "q":
                        P.ts("vector", y[0:M, :], ps[0:M, :], 0.125, None, ALU.mult)
                    else:
                        P.copy("vector", y[0:M, :], ps[0:M, :])
                    dst = d["FQ"] if gname[1] == "q" else d["FK"]
                    r0 = 0 if gname.endswith("01") else 128
                    P.dma(DQ2, dst[r0:r0 + M, t0:t0 + 512], y[0:M, :])
                elif gname[1] == "a":
                    glu_a_ps = ps
                else:
                    i = int(gname[2])
                    sg, u = sig[i], uu[i]
                    P.act(sg[:, :], ps[:, :], AF.Sigmoid)
                    P.tt("vector", u[:, :], glu_a_ps[:, :], sg[:, :], ALU.mult)
                    P.dma(DQ2, d["CU"][i * 128:(i + 1) * 128, 30 + t0:30 + t0 + 512], u[:, :])

            if "tm" in DBG:
                continue
            sm, st_, so = small[blk % 2], sm_t[blk % 2], sm_o[blk % 2]
            for tt in range(4):
                ps = tm_ps[n_tt % 2]
                g_bf, v_bf = gate_bf[n_tt % 2], fv_bf[n_tt % 2]
                n_tt += 1
                for k in range(8):
                    P.mm(ps[:, 0:NTM], xt[:, k, tt * 128:(tt + 1) * 128], w[:, k, TM0:TM0 + NTM],
                         start=(k == 0), stop=(k == 7))
                tok = slice(t0 + tt * 128, t0 + (tt + 1) * 128)
                if "nosm" not in DBG:
                    P.copy("vector", sm[:, tt, :], ps[:, 0:9])
                if "nogate" not in DBG:
                    P.act(g_bf[:, :], ps[:, 9:201], AF.Silu)
                    if "nogdma" not in DBG:
                        P.dma("sync", d["GATE"][tok, :], g_bf[:, :])
                if "nofv" not in DBG:
                    P.copy("vector", v_bf[:, :], ps[:, 201:393])
                    if "nofdma" not in DBG:
                        P.dma("sync", d["FV"][tok, :], v_bf[:, :])
            if "small" in DBG:
                continue
            P.tt("vector", st_[:, :, 0:3], sm[:, :, 0:3], dtb4[:, :, :], ALU.add)
            P.act(st_[:, :, 0:3], st_[:, :, 0:3], AF.Exp)
            P.act(st_[:, :, 0:3], st_[:, :, 0:3], AF.Ln, bias=1.0)
            P.tt("vector", so[:, :, 0:3], st_[:, :, 0:3], negA4[:, :, :], ALU.mult)
            P.act(st_[:, :, 3:6], sm[:, :, 3:6], AF.Exp, scale=-1.0)
            P.ts("vector", st_[:, :, 3:6], st_[:, :, 3:6], 1.0, None, ALU.add)
            P.recip(so[:, :, 3:6], st_[:, :, 3:6])
            P.tt("vector", st_[:, :, 6:9], sm[:, :, 6:9], fb4[:, :, :], ALU.add)
            P.act(st_[:, :, 6:9], st_[:, :, 6:9], AF.Exp, scale=-1.0)
            P.act(st_[:, :, 6:9], st_[:, :, 6:9], AF.Ln, bias=1.0)
            P.ts("vector", so[:, :, 6:9], st_[:, :, 6:9], -1.0, None, ALU.mult)
            P.dma("sync", d["SMALL"][t0:t0 + 512, :].rearrange("(t p) c -> p t c", p=128), so[:, :, :])


def pack_cpar(inp, l, hh):
    out = np.zeros((128, 2, 35), np.float32)
    perm = np.array(conv_perm(hh))
    for i in range(2):
        ch = perm[i * 128:(i + 1) * 128]
        out[:, i, 0:31] = inp["cnv_dw_w"][l][:, ch].T
        out[:, i, 31] = inp["cnv_dw_b"][l][ch]
        out[:, i, 32] = inp["cnv_ln_g"][l][ch]
        out[:, i, 33] = inp["cnv_ln_b"][l][ch]
        out[:, i, 34] = inp["mix_scale"][l][384 + ch]
    return out


def phase_C(nc, T, d):
    NB = T // 512
    with Phase(nc, "C") as P:
        ident = P.tile([128, 128], BF16, "ident")
        P.dma("sync", ident[:, :], d["ident_bf"])
        cpar = P.tile([128, 2, 35], F32, "cpar")
        P.dma("sync", cpar[:, :, :], d["cpar"])
        dg = P.tile([128, 2, CK, 128], BF16, "dg")
        for i in range(2):
            for k in range(CK):
                P.ts("vector", dg[:, i, k, :], ident[:, :], cpar[:, i, k:k + 1], None, ALU.mult)
        onesN = P.tile([128, 128], F32, "onesN")
        P.memset("vector", onesN[:, :], 1.0 / CONVW)
        epst = P.tile([128, 1], F32, "eps")
        P.memset("vector", epst[:, :], LN_EPS)
        cps = [P.psum([128, 512], F32, "cps") for _ in range(2)]
        mps = P.psum([128, 512], F32, "mps")
        qps = P.psum([128, 512], F32, "qps")
        uf = [P.tile([128, 542], F32, "uf") for _ in range(2)]
        ub = [P.tile([128, 542], BF16, "ub") for _ in range(2)]
        yt = [P.tile([128, 512], F32, "y") for _ in range(2)]
        ysq = [P.tile([128, 512], F32, "ysq") for _ in range(2)]
        mean = P.tile([128, 512], F32, "mean")
        t1 = P.tile([128, 512], F32, "t1")
        rstd = P.tile([128, 512], F32, "rstd")
        z = [P.tile([128, 512], F32, "z") for _ in range(2)]
        ob = [P.tile([128, 512], BF16, "ob") for _ in range(2)]
        for blk in range(NB):
            t0 = blk * 512
            for i in range(2):
                P.dma("sync", uf[i][:, :], d["CU"][i * 128:(i + 1) * 128, t0:t0 + 542])
                P.copy("gpsimd", ub[i][:, :], uf[i][:, :])
                for k in range(CK):
                    P.mm(cps[i][:, :], dg[:, i, k, :], ub[i][:, k:k + 512], start=(k == 0), stop=(k == CK - 1))
                P.act(yt[i][:, :], cps[i][:, :], AF.Identity, bias=cpar[:, i, 31:32])
                P.act(ysq[i][:, :], yt[i][:, :], AF.Square)
            for i in range(2):
                P.mm(mps[:, :], onesN[:, :], yt[i][:, :], start=(i == 0), stop=(i == 1))
            for i in range(2):
                P.mm(qps[:, :], onesN[:, :], ysq[i][:, :], start=(i == 0), stop=(i == 1))
            P.copy("scalar", mean[:, :], mps[:, :])
            P.stt("vector", t1[:, :], mean[:, :], -1.0, mean[:, :], ALU.mult, ALU.mult)
            P.tt("vector", t1[:, :], qps[:, :], t1[:, :], ALU.add)
            P.act(t1[:, :], t1[:, :], AF.Sqrt, bias=epst[:, 0:1])
            P.recip(rstd[:, :], t1[:, :])
            for i in range(2):
                P.tt("vector", z[i][:, :], yt[i][:, :], mean[:, :], ALU.subtract)
                P.tt("vector", z[i][:, :], z[i][:, :], rstd[:, :], ALU.mult)
                P.ts("vector", z[i][:, :], z[i][:, :], cpar[:, i, 32:33], cpar[:, i, 33:34], ALU.mult, ALU.add)
                P.act(z[i][:, :], z[i][:, :], AF.Silu)
                P.ts("vector", ob[i][:, :], z[i][:, :], cpar[:, i, 34:35], None, ALU.mult)
                P.dma(DQ2, d["MB"][i * 128:(i + 1) * 128, t0:t0 + 512], ob[i][:, :])


def make_consts():
    p = np.arange(128)
    c = {}
    c["ident_bf"] = np.eye(128, dtype=ml_dtypes.bfloat16)
    cf = np.zeros((128, 6, 128), np.float32)
    cf[:, 0, :] = (p[:, None] <= p[None, :])
    cf[:, 1, :] = 1.0
    cf[:, 2, :] = np.where(p[:, None] > p[None, :], -BIG, 0.0)
    cf[:, 3, :] = np.eye(128)
    cf[:, 4, :] = np.where(p[:, None] <= p[None, :], BIG, 0.0)
    cf[:, 5, :] = np.where(p[:, None] > p[None, :], -BIG, 0.0)
    c["cf32"] = cf
    sel = np.zeros((128, 64), np.float32)
    sel[64, :] = 1.0
    c["sel"] = sel
    return c


class DV(V):
    pass


def phase_D(nc, T, d):
    NBk = T // 128
    NI = T // 512
    with Phase(nc, "D") as P:
        cf = P.tile([128, 6, 128], F32, "cf")
        P.dma("sync", cf[:, :, :], d["cf32"])
        sel = P.tile([128, 64], F32, "sel")
        P.dma("sync", sel[:, :], d["sel"])
        fms = P.tile([64, 3], F32, "fms")
        P.dma("sync", fms[:, :], d["fms"])
        lf = P.tile([128, NBk, 3], F32, "lf")
        for j0 in range(0, NBk, 16):
            j1 = min(NBk, j0 + 16)
            P.dma("sync", lf[:, j0:j1, :], d["SMALL"][j0 * 128:j1 * 128, 6:9].rearrange("(j p) c -> p j c", p=128))
        lfh = P.tile([128, 3, NBk], F32, "lfh")
        for h in range(3):
            P.copy("vector", lfh[:, h, :], lf[:, :, h])
        setup_ps = P.psum([128, 512], F32, "sps")
        cw = P.tile([128, 3, NBk], F32, "cw")
        tot = P.tile([128, 3, NBk], F32, "tot")
        incl = P.tile([128, 3, NBk], F32, "incl")
        cc = P.tile([128, 3, NBk], F32, "cc")
        dl = P.tile([128, 3, NBk], F32, "dl")
        onesb = P.tile([128, NBk], F32, "onesb")
        P.memset("vector", onesb[:, :], 1.0)
        P.mm(setup_ps[:, 0:3 * NBk], cf[:, 0, :], lfh[:, :, :], start=True, stop=True)
        P.copy("vector", cw[:, :, :], setup_ps[:, 0:3 * NBk])
        P.mm(setup_ps[:, 0:3 * NBk], cf[:, 1, :], lfh[:, :, :], start=True, stop=True)
        P.copy("vector", tot[:, :, :], setup_ps[:, 0:3 * NBk])
        for h in range(3):
            P.scan(incl[:, h, :], onesb[:, :], tot[:, h, :], 0.0, ALU.mult, ALU.add)
        P.tt("vector", cc[:, :, :], cw[:, :, :], incl[:, :, :], ALU.add)
        P.tt("vector", cc[:, :, :], cc[:, :, :], tot[:, :, :], ALU.subtract)
        for h in range(3):
            for I in range(NI):
                P.ts("vector", dl[:, h, 4 * I:4 * I + 4], cc[:, h, 4 * I:4 * I + 4], incl[:, h, 4 * I + 3:4 * I + 4], None,
                     ALU.subtract)
        dlT = P.tile([128, 128], BF16, "dlT")
        drow = DV(d["DROW"], Res("drow"))
        for h in range(3):
            P.tr(setup_ps[0:NBk, 0:128], dl[:, h, :], cf[:, 3, :])
            P.copy("vector", dlT[0:NBk, :], setup_ps[0:NBk, 0:128])
            P.dma("sync", V(d["DROW"][h, :].rearrange("(j p) -> j p", p=128), drow.r), dlT[0:NBk, :])

        s_ps = [P.psum([128, 512], F32, "s") for _ in range(3)]
        o_ps = [P.psum([128, 512], F32, "o") for _ in range(3)]
        b_ps = P.psum([128, 512], F32, "b")
        pt = [[P.tile([128, 512], BF16, "pt") for _ in range(2)] for _ in range(3)]
        qa = [P.tile([65, T], BF16, "qa") for _ in range(3)]
        ka = [P.tile([65, T], BF16, "ka") for _ in range(3)]
        va = [P.tile([128, NBk, 65], BF16, "va") for _ in range(3)]
        bI = [[P.tile([128, NBk], F32, "bI") for _ in range(2)] for _ in range(3)]
        osb = [P.tile([65, 512], F32, "osb") for _ in range(3)]
        rec = [P.tile([64, 512], F32, "rec") for _ in range(3)]
        oo = [P.tile([64, 512], BF16, "oo") for _ in range(3)]
        for h in range(3):
            q, k, v = qa[h], ka[h], va[h]
            P.dma("sync", q[0:64, :], d["FQ"][h * 64:(h + 1) * 64, :])
            P.dma("sync", q[64:65, :], V(d["DROW"][h:h + 1, :], drow.r))
            P.dma("sync", k[0:64, :], d["FK"][h * 64:(h + 1) * 64, :])
            P.memset("vector", k[64:65, :], 1.0)
            for j0 in range(0, NBk, 16):
                j1 = min(NBk, j0 + 16)
                P.dma(DQ2, v[:, j0:j1, 0:64],
                      d["FV"][j0 * 128:j1 * 128, h * 64:(h + 1) * 64].rearrange("(j p) c -> p j c", p=128))
            P.memset("vector", v[:, :, 64:65], 1.0)

        def head(h):
            q, k, v = qa[h], ka[h], va[h]
            n_s = 0
            for I in range(NI):
                b = bI[h][I % 2]
                P.ts("vector", b[:, :], cc[:, h, :], -1.0, incl[:, h, 4 * I + 3:4 * I + 4], ALU.mult, ALU.add)
                ops_ = o_ps[h]
                jmax = 4 * I + 3
                for j in range(jmax + 1):
                    r = j - 4 * I
                    q0 = 128 * r if r > 0 else 0
                    sp = s_ps[h]
                    p_ = pt[h][n_s % 2]
                    n_s += 1
                    P.mm(sp[:, q0:512], k[0:65, 128 * j:128 * j + 128], q[0:65, 512 * I + q0:512 * I + 512],
                         start=True, stop=True)
                    yield
                    if r >= 0:
                        P.tt("vector", sp[:, q0:q0 + 128], sp[:, q0:q0 + 128], cf[:, 2, :], ALU.add)
                    P.act(p_[:, q0:512], sp[:, q0:512], AF.Exp, bias=b[:, j:j + 1])
                    yield
                    P.mm(ops_[0:65, q0:512], v[:, j, 0:65], p_[:, q0:512], start=(j == 0), stop=(j == jmax))
                ob_, rc, o_ = osb[h], rec[h], oo[h]
                P.copy("scalar", ob_[0:65, :], ops_[0:65, :])
                P.mm(b_ps[0:64, :], sel[0:65, :], ob_[0:65, :], start=True, stop=True)
                P.recip(rc[:, :], b_ps[0:64, :])
                P.stt("vector", o_[:, :], ob_[0:64, :], fms[:, h:h + 1], rc[:, :], ALU.mult, ALU.mult)
                P.dma(DQ2, d["MC"][h * 64:(h + 1) * 64, 512 * I:512 * I + 512], o_[:, :])
                yield

        gens = [head(h) for h in range(3)]
        for k_, g_ in enumerate(gens):
            for _ in range(k_):
                next(g_, None)
        live = list(gens)
        while live:
            for g_ in list(live):
                try:
                    next(g_)
                except StopIteration:
                    live.remove(g_)


def phase_B(nc, T, d):
    NCH = T // 128
    NMB = T // 512
    with Phase(nc, "B") as P:
        cf = P.tile([128, 6, 128], F32, "cf")
        P.dma("sync", cf[:, :, :], d["cf32"])
        U, ONES, IDF, PMASK, NMASK = cf[:, 0, :], cf[:, 1, :], cf[:, 3, :], cf[:, 4, :], cf[:, 5, :]
        ident = P.tile([128, 128], BF16, "ident")
        P.dma("sync", ident[:, :], d["ident_bf"])
        ones_bf = P.tile([128, 128], BF16, "ones_bf")
        P.memset("vector", ones_bf[:, :], 1.0)
        epsn = P.tile([128, 1], F32, "epsn")
        P.memset("vector", epsn[:, :], NORM_EPS)
        gnw = P.tile([128, 192], F32, "gnw")
        gms = P.tile([128, 192], F32, "gms")
        P.dma("sync", gnw[:, :], d["gnw"])
        P.dma("sync", gms[:, :], d["gms"])
        nwms = P.tile([128, 192], F32, "nwms")
        P.tt("vector", nwms[:, :], gnw[:, :], gms[:, :], ALU.mult)

        sm = P.tile([128, NCH, 6], F32, "sm")
        for j0 in range(0, NCH, 16):
            j1 = min(NCH, j0 + 16)
            P.dma("sync", sm[:, j0:j1, :], d["SMALL"][j0 * 128:j1 * 128, 0:6].rearrange("(j p) c -> p j c", p=128))
        gh = P.tile([128, 3, NCH], F32, "gh")
        bh = P.tile([128, 3, NCH], F32, "bh")
        for h in range(3):
            P.copy("vector", gh[:, h, :], sm[:, :, h])
            P.copy("vector", bh[:, h, :], sm[:, :, 3 + h])
        W = 3 * NCH
        set_ps = P.psum([128, 512], F32, "set")
        gc = P.tile([128, 3, NCH], F32, "gc")
        gtot = P.tile([128, 3, NCH], F32, "gtot")
        P.mm(set_ps[:, 0:W], U, gh[:, :, :])
        P.copy("vector", gc[:, :, :], set_ps[:, 0:W])
        P.mm(set_ps[:, 0:W], ONES, gh[:, :, :])
        P.copy("vector", gtot[:, :, :], set_ps[:, 0:W])
        negc = P.tile([128, 3, NCH], F32, "negc")
        negb = P.tile([128, 3, NCH], F32, "negb")
        bege = P.tile([128, 3, NCH], F32, "bege")
        kdsc = P.tile([128, 3, NCH], F32, "kdsc")
        egtot = P.tile([128, 3, NCH], F32, "egtot")
        P.ts("vector", negc[:, :, :], gc[:, :, :], -1.0, None, ALU.mult)
        P.ts("vector", negb[:, :, :], bh[:, :, :], -1.0, None, ALU.mult)
        P.act(bege[:, :, :], gc[:, :, :], AF.Exp)
        P.tt("vector", bege[:, :, :], bege[:, :, :], bh[:, :, :], ALU.mult)
        P.tt("vector", kdsc[:, :, :], gtot[:, :, :], gc[:, :, :], ALU.subtract)
        P.act(kdsc[:, :, :], kdsc[:, :, :], AF.Exp)
        P.act(egtot[:, :, :], gtot[:, :, :], AF.Exp)

        NL_ = 4
        bk = [[set_ps if (l_ == 0 and j_ == 0) else P.psum([128, 512], F32, f"bk{l_}{j_}") for j_ in range(2)]
              for l_ in range(NL_)]
        R2 = 6
        def rot(shape, dt, name, n=R2):
            return [P.tile(shape, dt, name) for _ in range(n)]
        qf, kf, vf = rot([64, 512], BF16, "qf", 6), rot([64, 512], BF16, "kf", 6), rot([64, 512], BF16, "vf", 6)
        sqt = rot([64, 512], BF16, "sqt", 2)
        rn = rot([64, 512], F32, "rn", 2)
        qn, kn = rot([64, 512], BF16, "qn", 6), rot([64, 512], BF16, "kn", 6)
        gate = rot([128, 192], BF16, "gate", 2)
        gw = rot([128, 192], F32, "gw", 2)
        G = rot([128, 128], F32, "G")
        dstr = rot([128, 128], F32, "dstr")
        decT = rot([128, 128], F32, "decT")
        egcb = rot([64, 128], F32, "egcb")
        Nt = [rot([128, 128], BF16, "N") for _ in range(2)]
        Mt = [rot([128, 128], BF16, "M") for _ in range(2)]
        Pf = rot([128, 128], F32, "Pf")
        Pb = rot([128, 128], BF16, "Pb")
        ktok, vtok = rot([128, 64], BF16, "ktok"), rot([128, 64], BF16, "vtok")
        vb, kbg, kd = rot([128, 64], BF16, "vb"), rot([128, 64], BF16, "kbg"), rot([128, 64], BF16, "kd")
        u_sb = rot([128, 64], F32, "u")
        wT = rot([64, 128], BF16, "wT")
        AT = rot([128, 128], BF16, "AT")
        qg = rot([64, 128], BF16, "qg")
        vnew = rot([128, 64], BF16, "vnew")
        Sf = [P.tile([64, 64], F32, "Sf") for _ in range(3)]
        Sb = [P.tile([64, 64], BF16, "Sb") for _ in range(3)]
        for h in range(3):
            P.memset("vector", Sf[h][:, :], 0.0)
            P.memset("vector", Sb[h][:, :], 0.0)
        junk = rot([128, 64], F32, "junk")
        ss = rot([128, 1], F32, "ss")
        rstd = rot([128, 1], F32, "rstd")
        on = rot([128, 64], BF16, "on")
        oT = [P.tile([64, 128], BF16, "oT") for _ in range(6)]

        u_i = 0
        WIN = NL_
        active = []
        n_units = [0]

        def rr_pass():
            for g_ in list(active):
                try:
                    next(g_)
                except StopIteration:
                    active.remove(g_)

        for mb in range(NMB):
            t0 = mb * 512
            for h in range(3):
                x = (mb % 2) * 3 + h
                q_, k_, v_ = qf[x], kf[x], vf[x]
                P.dma("sync", q_[:, :], d["GQ"][h * 3 + 0, :, t0:t0 + 512])
                P.dma("sync", k_[:, :], d["GQ"][h * 3 + 1, :, t0:t0 + 512])
                P.dma("sync", v_[:, :], d["GQ"][h * 3 + 2, :, t0:t0 + 512])
                for src, dst, scl in ((q_, qn[x], HD ** -0.5), (k_, kn[x], 1.0)):
                    s2, r_ = sqt[u_i % 2], rn[u_i % 2]
                    sp = bk[u_i % NL_][0]
                    u_i += 1
                    P.tt("vector", s2[:, :], src[:, :], src[:, :], ALU.mult)
                    P.mm(sp[0:64, :], ones_bf[0:64, 0:64], s2[:, :])
                    P.act(r_[:, :], sp[0:64, :], AF.Sqrt, bias=epsn[0:64, 0:1])
                    P.recip(r_[:, :], r_[:, :])
                    P.stt("vector", dst[:, :], src[:, :], scl, r_[:, :], ALU.mult, ALU.mult)
            for c in range(4):
                n = mb * 4 + c
                cs = slice(c * 128, (c + 1) * 128)
                gt, gw_ = gate[n % 2], gw[n % 2]
                while len(active) > 3:
                    rr_pass()
                P.dma(DQ2, gt[:, :], d["GATE"][n * 128:(n + 1) * 128, :])
                P.tt("gpsimd", gw_[:, :], gt[:, :], nwms[:, :], ALU.mult)
                def unit(h, lane, n=n, mb=mb, cs=cs, gw_=gw_):
                        b0, b1 = bk[lane]
                        x = (mb % 2) * 3 + h
                        y = h * 2 + n % 2
                        qn_, kn_, v_ = qn[x], kn[x], vf[x]
                        gcol = gh[:, h, n:n + 1]
                        P.ts("gpsimd", G[y][:, :], ONES, gcol, None, ALU.mult)
                        P.mm(b0[:, 0:128], G[y][:, :], U, start=True, stop=True)
                        P.mm(b0[:, 128:256], G[y][:, :], U, start=True, stop=False)
                        P.mm(b0[:, 128:256], IDF, PMASK, start=False, stop=True)
                        P.mm(b0[:, 256:384], G[y][:, :], U, start=True, stop=False)
                        P.mm(b0[:, 256:384], IDF, NMASK, start=False, stop=True)
                        P.act(egcb[y][:, :], b0[0:64, 0:128], AF.Exp)
                        P.act(dstr[y][:, :], b0[:, 128:256], AF.Exp, scale=-1.0, bias=gc[:, h, n:n + 1])
                        P.act(decT[y][:, :], b0[:, 256:384], AF.Exp, bias=negc[:, h, n:n + 1])
                        yield
                        P.mm(b1[:, 0:64], kn_[:, cs], ident[0:64, 0:64])
                        P.mm(b1[:, 64:128], v_[:, cs], ident[0:64, 0:64])
                        P.copy("vector", ktok[y][:, :], b1[:, 0:64])
                        P.copy("scalar", vtok[y][:, :], b1[:, 64:128])
                        P.ts("gpsimd", vb[y][:, :], vtok[y][:, :], bh[:, h, n:n + 1], None, ALU.mult)
                        P.ts("gpsimd", kbg[y][:, :], ktok[y][:, :], bege[:, h, n:n + 1], None, ALU.mult)
                        P.ts("gpsimd", kd[y][:, :], ktok[y][:, :], kdsc[:, h, n:n + 1], None, ALU.mult)
                        yield
                        P.mm(b0[:, 0:128], kn_[:, cs], kn_[:, cs])
                        P.mm(b0[:, 128:256], kn_[:, cs], qn_[:, cs])
                        N0, M0 = Nt[0][y], Mt[0][y]
                        P.stt("vector", N0[:, :], b0[:, 0:128], negb[:, h, n:n + 1], dstr[y][:, :], ALU.mult, ALU.mult)
                        P.tt("vector", AT[y][:, :], b0[:, 128:256], decT[y][:, :], ALU.mult)
                        P.mm(b1[:, 128:256], N0[:, :], ident[:, :])
                        P.copy("scalar", M0[:, :], b1[:, 128:256])
                        yield
                        P.tt("vector", Pf[y][:, :], M0[:, :], IDF, ALU.add)
                        P.copy("scalar", Pb[y][:, :], Pf[y][:, :])
                        cur = 0
                        for rnd in range(0, 7):
                            Nc, Mc = Nt[cur][y], Mt[cur][y]
                            Nn, Mn = Nt[1 - cur][y], Mt[1 - cur][y]
                            sp = bk[lane][rnd % 2]
                            pp = bk[lane][1 - rnd % 2]
                            if rnd < 6:
                                P.mm(sp[:, 0:128], Mc[:, :], Nc[:, :])
                                if rnd < 5:
                                    P.mm(sp[:, 128:256], Nc[:, :], Mc[:, :])
                            if rnd >= 1:
                                P.mm(pp[:, 256:384], Nc[:, :], Pb[y][:, :])
                            if rnd < 6:
                                P.copy("scalar", Nn[:, :], sp[:, 0:128])
                                if rnd < 5:
                                    P.copy("vector", Mn[:, :], sp[:, 128:256])
                            if rnd >= 1:
                                P.tt("vector", Pf[y][:, :], Pf[y][:, :], pp[:, 256:384], ALU.add)
                                P.copy("scalar", Pb[y][:, :], Pf[y][:, :])
                            cur = 1 - cur
                            yield
                        P.mm(b0[:, 256:320], Pb[y][:, :], vb[y][:, :])
                        P.mm(b0[0:64, 384:512], kbg[y][:, :], Pb[y][:, :])
                        P.copy("vector", u_sb[y][:, :], b0[:, 256:320])
                        P.copy("scalar", wT[y][:, :], b0[0:64, 384:512])
                        P.tt("gpsimd", qg[y][:, :], qn_[:, cs], egcb[y][:, :], ALU.mult)
                        yield
                        P.mm(b1[:, 0:64], wT[y][:, :], Sb[h][:, :])
                        P.tt("vector", vnew[y][:, :], u_sb[y][:, :], b1[:, 0:64], ALU.subtract)
                        P.mm(b1[:, 64:128], qg[y][:, :], Sb[h][:, :], start=True, stop=False)
                        P.mm(b1[:, 64:128], AT[y][:, :], vnew[y][:, :], start=False, stop=True)
                        P.mm(b1[0:64, 128:192], kd[y][:, :], vnew[y][:, :])
                        P.stt("vector", Sf[h][:, :], Sf[h][:, :], egtot[0:64, h, n:n + 1], b1[0:64, 128:192], ALU.mult, ALU.add)
                        P.copy("scalar", Sb[h][:, :], Sf[h][:, :])
                        P.act(junk[y][:, :], b1[:, 64:128], AF.Square, accum=ss[y][:, 0:1])
                        P.act(rstd[y][:, :], ss[y][:, :], AF.Sqrt, scale=1.0 / HD, bias=epsn[:, 0:1])
                        P.recip(rstd[y][:, :], rstd[y][:, :])
                        P.stt("vector", on[y][:, :], b1[:, 64:128], rstd[y][:, 0:1], gw_[:, h * 64:(h + 1) * 64], ALU.mult, ALU.mult)
                        P.mm(b0[0:64, 0:128], on[y][:, :], ident[:, :])
                        P.copy("scalar", oT[y][:, :], b0[0:64, 0:128])
                        P.dma(DQ2, d["MA"][h * 64:(h + 1) * 64, n * 128:(n + 1) * 128], oT[y][:, :])

                for h in range(3):
                    while len(active) >= WIN:
                        rr_pass()
                    active.append(unit(h, n_units[0] % NL_))
                    n_units[0] += 1
        while active:
            rr_pass()


def _layernorm_tile(P, y, g_bc, b_bc, out, st, mv, rstd, epst):
    P.S.op("vector", (lambda o, i: (lambda e: e.bn_stats(out=o, in_=i)))(st[:, 0:6].ap, y[:, 0:512].ap),
           reads=_rs(y[:, :]), writes=_rs(st[:, :]))
    P.S.op("vector", (lambda o, i: (lambda e: e.bn_stats(out=o, in_=i)))(st[:, 6:12].ap, y[:, 512:1024].ap),
           reads=_rs(y[:, :]), writes=_rs(st[:, :]))
    P.S.op("vector", (lambda o, i: (lambda e: e.bn_aggr(out=o, in_=i)))(mv[:, 0:2].ap, st[:, 0:12].ap),
           reads=_rs(st[:, :]), writes=_rs(mv[:, :]))
    P.act(rstd[:, :], mv[:, 1:2], AF.Sqrt, bias=epst[:, 0:1])
    P.recip(rstd[:, :], rstd[:, :])
    P.ts("vector", out[:, :], y[:, :], mv[:, 0:1], rstd[:, 0:1], ALU.subtract, ALU.mult)
    P.tt("gpsimd", out[:, :], out[:, :], g_bc[:, :], ALU.mult)
    P.tt("gpsimd", out[:, :], out[:, :], b_bc[:, :], ALU.add)


WO_PIECES = [("MA", 0, 128), ("MA", 128, 64), ("MC", 0, 128), ("MC", 128, 64), ("MB", 0, 128)]


def pack_w_out(w_out_l, hh):
    out = np.zeros((5, 128, D), np.float32)
    bases = {"MA": 192 * hh, "MC": 640 + 192 * hh, "MB": 384 + 128 * hh}
    for i, (nm, r0, n) in enumerate(WO_PIECES):
        out[i, :n] = w_out_l[bases[nm] + r0: bases[nm] + r0 + n]
    return out


def phase_E1(nc, T, d, moe, xres_name, groups):
    NT = T // 128
    with Phase(nc, "E1") as P:
        CH, src_res, dst_res = _cc_chunks(P, T, d, "PART", "SUM", groups)
        NCHK = len(src_res)
        TPC = CH // 128
        wo = P.tile([128, 5, D], BF16, "wo")
        stg = [P.tile([128, D], F32, "stg") for _ in range(2)]
        for i in range(5):
            P.dma("sync", stg[i % 2][:, :], d["w_out"][i])
            P.copy(["vector", "gpsimd"][i % 2], wo[:, i, :], stg[i % 2][:, :])
        m_ps = [P.psum([128, 512], F32, "m") for _ in range(4)]
        mixT = [P.tile([128, 5, 512], BF16, "mixT") for _ in range(2)]
        po = [P.tile([128, D], F32, "po") for _ in range(2)]
        ident = P.tile([128, 128], BF16, "ident")
        P.dma("sync", ident[:, :], d["ident_bf"])
        g_bc = P.tile([128, D], F32, "g_bc")
        b_bc = P.tile([128, D], F32, "b_bc")
        P.dma("sync", g_bc[:, :], d["lnm_g"])
        P.dma("sync", b_bc[:, :], d["lnm_b"])
        epst = P.tile([128, 1], F32, "eps")
        P.memset("vector", epst[:, :], LN_EPS)
        if moe:
            wr_bc = P.tile([128, NEXP, D], F32, "wr_bc")
            P.dma("sync", wr_bc[:, :, :], d["w_router_bc"])
            rjunk = P.tile([128, D], F32, "rjunk")
        t_ps = [P.psum([128, 8, 128], BF16, "t") for _ in range(2)]
        xres = [P.tile([128, D], F32, "xres") for _ in range(2)]
        msum = [P.tile([128, D], F32, "msum") for _ in range(2)]
        y = [P.tile([128, D], F32, "y") for _ in range(2)]
        xm = [P.tile([128, D], F32, "xm") for _ in range(2)]
        xmb = [P.tile([128, D], BF16, "xmb") for _ in range(2)]
        xmT = [P.tile([128, 8, 128], BF16, "xmT") for _ in range(2)]
        st = [P.tile([128, 12], F32, "st") for _ in range(2)]
        mv = [P.tile([128, 2], F32, "mv") for _ in range(2)]
        rstd = [P.tile([128, 1], F32, "rstd") for _ in range(2)]
        if moe:
            lg = [P.tile([128, NEXP], F32, "lg") for _ in range(2)]
            sc = [P.tile([128, 4 * NEXP + 8], F32, "sc") for _ in range(2)]
            gt = [P.tile([128, NEXP], F32, "gt") for _ in range(2)]

        def part_a(ci):
            for blk in range(ci * CH // 512, (ci + 1) * CH // 512):
                mt = mixT[blk % 2]
                for pi, (nm, r0, n) in enumerate(WO_PIECES):
                    P.dma("sync", mt[0:n, pi, :], d[nm][r0:r0 + n, blk * 512:(blk + 1) * 512])
                for tt in range(4):
                    i = blk * 4 + tt
                    a = i % 2
                    for half in range(2):
                        ps = m_ps[(2 * i + half) % 4]
                        hs = slice(half * 512, (half + 1) * 512)
                        for pi, (nm, r0, n) in enumerate(WO_PIECES):
                            P.mm(ps[:, :], mt[0:n, pi, tt * 128:(tt + 1) * 128], wo[0:n, pi, hs], start=(pi == 0), stop=(pi == 4))
                        P.copy(["vector", "scalar"][half], po[a][:, hs], ps[:, :])
                    P.dma("sync", V(d["PART"][i * 128:(i + 1) * 128, :], src_res[ci]), po[a][:, :])

        def part_b(ci):
            for i in range(ci * TPC, (ci + 1) * TPC):
                a = i % 2
                tok = slice(i * 128, (i + 1) * 128)
                P.dma("sync", xres[a][:, :], d[xres_name][tok, :])
                P.dma("sync", msum[a][:, :], V(d["SUM"][tok, :], dst_res[ci]))
                P.stt("vector", y[a][:, :], xres[a][:, :], ALPHA, msum[a][:, :], ALU.mult, ALU.add)
                _layernorm_tile(P, y[a], g_bc, b_bc, xm[a], st[a], mv[a], rstd[a], epst)
                P.dma("sync", d["XMID"][tok, :], xm[a][:, :])
                P.copy("gpsimd", xmb[a][:, :], xm[a][:, :])
                for k in range(8):
                    P.tr(t_ps[a][:, k, :], xmb[a][:, k * 128:(k + 1) * 128], ident[:, :])
                P.copy("scalar", xmT[a][:, :, :], t_ps[a][:, :, :])
                P.dma("sync", d["XMT"][:, :, tok].rearrange("k p t -> p k t"), xmT[a][:, :, :])
                if moe:
                    L, s_, g_ = lg[a], sc[a], gt[a]
                    for e_ in range(NEXP):
                        P.stt("vector", rjunk[:, :], xm[a][:, :], 1.0, wr_bc[:, e_, :], ALU.mult, ALU.mult,
                              accum=L[:, e_:e_ + 1])
                    m1, m2, nm1, dm = s_[:, 32:33], s_[:, 33:34], s_[:, 34:35], s_[:, 35:36]
                    eq1, l2, sel, ex = s_[:, 0:8], s_[:, 8:16], s_[:, 16:24], s_[:, 24:32]
                    P.S.op("vector", (lambda o, i_: (lambda e: e.reduce_max(out=o, in_=i_, axis=AX.X)))(m1.ap, L[:, :].ap),
                           reads=_rs(L[:, :]), writes=_rs(m1))
                    P.ts("vector", eq1, L[:, :], m1, None, ALU.is_equal)
                    P.stt("vector", l2, eq1, -BIG, L[:, :], ALU.mult, ALU.add)
                    P.S.op("vector", (lambda o, i_: (lambda e: e.reduce_max(out=o, in_=i_, axis=AX.X)))(m2.ap, l2.ap),
                           reads=_rs(l2), writes=_rs(m2))
                    P.ts("vector", sel, L[:, :], m2, None, ALU.is_ge)
                    P.ts("vector", nm1, m1, -1.0, None, ALU.mult)
                    P.act(ex, L[:, :], AF.Exp, bias=nm1)
                    P.tt("vector", dm, m2, m1, ALU.subtract)
                    P.act(dm, dm, AF.Exp)
                    P.ts("vector", dm, dm, 1.0, None, ALU.add)
                    P.recip(dm, dm)
                    P.tt("vector", g_[:, :], ex, sel, ALU.mult)
                    P.ts("vector", g_[:, :], g_[:, :], dm, None, ALU.mult)
                    P.dma("sync", d["GATES"][:, i, :], g_[:, :])

        LAG = 2
        for step in range(NCHK + LAG):
            if step < NCHK:
                part_a(step)
                _cc_issue(P, d, "PART", "SUM", groups, step * CH, min(CH, T - step * CH), src_res[step], dst_res[step])
            if step >= LAG:
                part_b(step - LAG)


def phase_E2(nc, T, d, moe, sfx, hh_experts=None):
    NT = T // 128
    NBK = T // 512
    NE = NEXP // 2 if moe else 1
    F = FFN_EXP if moe else FFN_DENSE // 2
    NFH = 4 if moe else 1
    FHW = F // NFH
    FT = FHW // 128
    passes = [(e, fh) for e in range(NE) for fh in range(NFH)]
    NSET = 2 if len(passes) > 1 else 1
    with Phase(nc, "E2") as P:
        wset = [dict(w1=P.tile([128, 8, FHW], BF16, "w1h"), w3=P.tile([128, 8, FHW], BF16, "w3h"),
                     w2=P.tile([128, FT, D], BF16, "w2h")) for _ in range(NSET)]
        NSTG = 4
        stg = [P.tile([128, 2048], F32, "stg") for _ in range(NSTG)]
        gates = None
        if moe:
            gates = P.tile([128, NT, NEXP], F32, "gates")
            P.dma("sync", gates[:, :, :], d["GATES"])
        h_ps = [P.psum([128, 512], F32, "h") for _ in range(4)]
        y_ps = [P.psum([128, 512], F32, "y") for _ in range(2)]
        xmT = [P.tile([128, 8, 512], BF16, "xmT") for _ in range(2)]
        hT = P.tile([128, FT, 512], BF16, "hT")
        sg = [P.tile([128, 512], F32, "sg") for _ in range(2)]
        accL = [P.tile([128, D], F32, "accL") for _ in range(2)]
        accS = [P.tile([128, D], F32, "accS") for _ in range(2)]
        acc_res = [Res(f"acc{i}") for i in range(NT)]

        pieces = [("w1", k) for k in range(8)] + [("w3", k) for k in range(8)] + [("w2", ft) for ft in range(0, FT, 2)]
        n_c = [0]

        def piece_dma(pi, j):
            e, fh = passes[pi]
            f0 = fh * FHW
            kind, k = pieces[j]
            st_ = stg[n_c[0] % NSTG]
            n_c[0] += 1
            if kind in ("w1", "w3"):
                P.dma("sync", st_[:, 0:FHW], d[kind + sfx][e, k * 128:(k + 1) * 128, f0:f0 + FHW])
                return (st_, kind, k, 0)
            nn = min(2, FT - k)
            sv = V(st_.t[:, 0:nn * D].rearrange("p (a c) -> p a c", a=nn), st_.r)
            P.dma("sync", sv, d["w2" + sfx][e, f0 + k * 128:f0 + (k + nn) * 128, :].rearrange("(a p) c -> p a c", p=128))
            return (st_, kind, k, nn)

        def piece_cast(pi, pend, eng):
            st_, kind, k, nn = pend
            ws = wset[pi % NSET]
            if kind in ("w1", "w3"):
                P.copy(eng, ws[kind][:, k, :], st_[:, 0:FHW])
            else:
                P.copy(eng, V(ws["w2"].t[:, k:k + nn, :], ws["w2"].r),
                       V(st_.t[:, 0:nn * D].rearrange("p (a c) -> p a c", a=nn), st_.r))

        ceng = ["vector", "gpsimd", "scalar"]
        for j in range(len(pieces)):
            piece_cast(0, piece_dma(0, j), ceng[j % 3])

        n_h = 0
        n_y = 0
        n_b = 0
        first = True
        for pi, (e, fh) in enumerate(passes):
            ws = wset[pi % NSET]
            w1h, w3h, w2h = ws["w1"], ws["w3"], ws["w2"]
            pending = []
            nxt = 0
            for blk in range(NBK):
                xt = xmT[n_b % 2]
                n_b += 1
                P.dma("sync", xt[:, :, :], d["XMT"][:, :, blk * 512:(blk + 1) * 512].rearrange("k p t -> p k t"))
                if pi + 1 < len(passes):
                    for pend in pending:
                        piece_cast(pi + 1, pend, "scalar")
                    pending = []
                    tgt = (len(pieces) * (blk + 1) + NBK - 2) // (NBK - 1) if NBK > 1 else len(pieces)
                    tgt = min(len(pieces), tgt)
                    while nxt < tgt and len(pending) < NSTG - 1:
                        pending.append(piece_dma(pi + 1, nxt))
                        nxt += 1
                for ft in range(FT):
                    p1, p3 = h_ps[n_h % 4], h_ps[(n_h + 1) % 4]
                    s_ = sg[(n_h // 2) % 2]
                    n_h += 2
                    fs = slice(ft * 128, (ft + 1) * 128)
                    for k in range(8):
                        P.mm(p1[:, :], w1h[:, k, fs], xt[:, k, :], start=(k == 0), stop=(k == 7))
                    for k in range(8):
                        P.mm(p3[:, :], w3h[:, k, fs], xt[:, k, :], start=(k == 0), stop=(k == 7))
                    P.act(s_[:, :], p1[:, :], AF.Silu)
                    P.tt("vector", hT[:, ft, :], s_[:, :], p3[:, :], ALU.mult)
                for tt in range(4):
                    i = blk * 4 + tt
                    tok = slice(i * 128, (i + 1) * 128)
                    aL, aS = accL[i % 2], accS[i % 2]
                    accd = V(d["PART"][tok, :], acc_res[i])
                    if not first:
                        P.dma(DQ2, aL[:, :], accd)
                    for half in range(2):
                        yp = y_ps[n_y % 2]
                        n_y += 1
                        hs = slice(half * 512, (half + 1) * 512)
                        for ft in range(FT):
                            P.mm(yp[:, :], hT[:, ft, tt * 128:(tt + 1) * 128], w2h[:, ft, hs],
                                 start=(ft == 0), stop=(ft == FT - 1))
                        if first:
                            if moe:
                                P.ts("vector", aS[:, hs], yp[:, :], gates[:, i, e:e + 1], None, ALU.mult)
                            else:
                                P.copy("vector", aS[:, hs], yp[:, :])
                        else:
                            if moe:
                                P.stt("vector", aS[:, hs], yp[:, :], gates[:, i, e:e + 1], aL[:, hs], ALU.mult, ALU.add)
                            else:
                                P.tt("vector", aS[:, hs], yp[:, :], aL[:, hs], ALU.add)
                    P.dma(DQ2, accd, aS[:, :])
            if pi + 1 < len(passes):
                for pend in pending:
                    piece_cast(pi + 1, pend, "scalar")
                while nxt < len(pieces):
                    piece_cast(pi + 1, piece_dma(pi + 1, nxt), "scalar")
                    nxt += 1
            first = False


def _cc_chunks(P, T, d, src, dst, groups):
    CH = 1024
    src_res, dst_res = [], []
    for i in range(0, T, CH):
        n = min(CH, T - i)
        src_res.append(Res(f"ccs{i}"))
        dst_res.append(Res(f"ccd{i}"))
    return CH, src_res, dst_res


def _cc_issue(P, d, src, dst, groups, i0, n, rs, rd):
    a, b = d[src][i0:i0 + n, :], d[dst][i0:i0 + n, :]
    P.S.op("gpsimd", (lambda a_, b_: (lambda e: e.collective_compute(
        "AllReduce", ALU.add, replica_groups=groups, ins=[a_.opt()], outs=[b_.opt()])))(a, b),
        reads=[rs], writes=[rd], cc=True)


def phase_E3(nc, T, d, sfx, out_name, xt_name, groups=None):
    NT = T // 128
    with Phase(nc, "E3") as P:
        CH, src_res, dst_res = _cc_chunks(P, T, d, "PART", "SUM", groups)
        if groups is not None:
            for ci in range(len(src_res)):
                _cc_issue(P, d, "PART", "SUM", groups, ci * CH, min(CH, T - ci * CH), src_res[ci], dst_res[ci])
        ident = P.tile([128, 128], BF16, "ident")
        P.dma("sync", ident[:, :], d["ident_bf"])
        g_bc = P.tile([128, D], F32, "g_bc")
        b_bc = P.tile([128, D], F32, "b_bc")
        P.dma("sync", g_bc[:, :], d["lnf_g" + sfx])
        P.dma("sync", b_bc[:, :], d["lnf_b" + sfx])
        epst = P.tile([128, 1], F32, "eps")
        P.memset("vector", epst[:, :], LN_EPS)
        t_ps = [P.psum([128, 8, 128], BF16, "t") for _ in range(2)]
        xm = [P.tile([128, D], F32, "xm") for _ in range(2)]
        ac = [P.tile([128, D], F32, "ac") for _ in range(2)]
        y = [P.tile([128, D], F32, "y") for _ in range(2)]
        o = [P.tile([128, D], F32, "o") for _ in range(2)]
        ob = [P.tile([128, D], BF16, "ob") for _ in range(2)]
        oT = [P.tile([128, 8, 128], BF16, "oT") for _ in range(2)]
        st = [P.tile([128, 12], F32, "st") for _ in range(2)]
        mv = [P.tile([128, 2], F32, "mv") for _ in range(2)]
        rstd = [P.tile([128, 1], F32, "rstd") for _ in range(2)]
        for i in range(NT):
            a = i % 2
            tok = slice(i * 128, (i + 1) * 128)
            P.dma("sync", xm[a][:, :], d["XMID"][tok, :])
            P.dma("sync", ac[a][:, :], V(d["SUM"][tok, :], dst_res[(i * 128) // CH]))
            P.stt("vector", y[a][:, :], xm[a][:, :], ALPHA, ac[a][:, :], ALU.mult, ALU.add)
            _layernorm_tile(P, y[a], g_bc, b_bc, o[a], st[a], mv[a], rstd[a], epst)
            P.dma(DQ2, d[out_name][tok, :], o[a][:, :])
            if xt_name is not None:
                P.copy("gpsimd", ob[a][:, :], o[a][:, :])
                for k in range(8):
                    P.tr(t_ps[a][:, k, :], ob[a][:, k * 128:(k + 1) * 128], ident[:, :])
                P.copy("scalar", oT[a][:, :, :], t_ps[a][:, :, :])
                P.dma(DQ2, d[xt_name][:, :, tok].rearrange("k p t -> p k t"), oT[a][:, :, :])


MIX_PARAMS = ["w_in", "gcw", "bc9", "fms", "cpar", "gnw", "gms"]
TOK_PARAMS = ["w_out", "lnm_g", "lnm_b", "lnf_g", "lnf_b", "w1", "w3", "w2"]


class Prog:
    def __init__(self):
        self.nc = bass.Bass("TRN2", target_bir_lowering=False)
        self.d = {}

    def I(self, n, shape, dt):
        self.d[n] = self.nc.dram_tensor(n, list(shape), dt, kind="ExternalInput").ap()

    def O(self, n, shape, dt):
        self.d[n] = self.nc.dram_tensor(n, list(shape), dt, kind="ExternalOutput").ap()

    def S(self, n, shape, dt):
        self.d[n] = self.nc.dram_tensor(n, list(shape), dt, kind="Internal").ap()


def expert_order(hh):
    return list(range(4 * hh, 4 * hh + 4)) + list(range(4 * (1 - hh), 4 * (1 - hh) + 4))


def core_inputs(inp, b, hh, consts):
    rep = lambda v: np.ascontiguousarray(np.tile(np.asarray(v, np.float32)[None], (128,) + (1,) * np.ndim(v)))
    m = dict(x=np.ascontiguousarray(inp["x"][b]), ident_bf=consts["ident_bf"], cf32=consts["cf32"], sel=consts["sel"])
    for l in range(2):
        ms = inp["mix_scale"][l]
        bc9 = np.concatenate([inp["gdn_dt_bias"][l, 3 * hh:3 * hh + 3], inp["gdn_a_log"][l, 3 * hh:3 * hh + 3],
                              inp["fox_f_bias"][l, 3 * hh:3 * hh + 3]]).astype(np.float32)
        m[f"w_in_{l}"] = pack_w_in(inp["w_in"][l], hh)
        m[f"gcw_{l}"] = pack_gcw(inp["gdn_conv_w"][l], hh)
        m[f"bc9_{l}"] = rep(bc9)
        m[f"fms_{l}"] = np.ascontiguousarray(ms[640 + 192 * hh:640 + 192 * hh + 192].reshape(3, 64).T)
        m[f"cpar_{l}"] = pack_cpar(inp, l, hh)
        m[f"gnw_{l}"] = rep(np.tile(inp["gdn_norm_w"][l], 3))
        m[f"gms_{l}"] = rep(ms[192 * hh:192 * hh + 192])
        m[f"w_out_{l}"] = pack_w_out(inp["w_out"][l], hh)
        m[f"lnm_g_{l}"] = rep(inp["ln_mix_g"][l])
        m[f"lnm_b_{l}"] = rep(inp["ln_mix_b"][l])
        m[f"lnf_g_{l}"] = rep(inp["ln_ffn_g"][l])
        m[f"lnf_b_{l}"] = rep(inp["ln_ffn_b"][l])
    fh = FFN_DENSE // 2
    m["w1_0"] = np.ascontiguousarray(inp["ffn_w1"][0:1, :, hh * fh:(hh + 1) * fh])
    m["w3_0"] = np.ascontiguousarray(inp["ffn_w3"][0:1, :, hh * fh:(hh + 1) * fh])
    m["w2_0"] = np.ascontiguousarray(inp["ffn_w2"][0:1, hh * fh:(hh + 1) * fh, :])
    es = slice(4 * hh, 4 * hh + 4)
    m["w1_1"] = np.ascontiguousarray(inp["moe_w1"][0, es])
    m["w3_1"] = np.ascontiguousarray(inp["moe_w3"][0, es])
    m["w2_1"] = np.ascontiguousarray(inp["moe_w2"][0, es])
    m["w_router_bc"] = rep(np.ascontiguousarray(inp["moe_router"][0].T[expert_order(hh)]))
    return m


def build_fused(T, groups):
    p = Prog()
    NT = T // 128
    fh = FFN_DENSE // 2
    p.I("x", [T, D], F32)
    p.I("ident_bf", [128, 128], BF16)
    p.I("cf32", [128, 6, 128], F32)
    p.I("sel", [128, 64], F32)
    for l in range(2):
        p.I(f"w_in_{l}", [D, NCOL], F32)
        p.I(f"gcw_{l}", [128, 9, 4], F32)
        p.I(f"bc9_{l}", [128, 9], F32)
        p.I(f"fms_{l}", [64, 3], F32)
        p.I(f"cpar_{l}", [128, 2, 35], F32)
        p.I(f"gnw_{l}", [128, 192], F32)
        p.I(f"gms_{l}", [128, 192], F32)
        p.I(f"w_out_{l}", [5, 128, D], F32)
        for n in ("lnm_g", "lnm_b", "lnf_g", "lnf_b"):
            p.I(f"{n}_{l}", [128, D], F32)
    p.I("w1_0", [1, D, fh], F32)
    p.I("w3_0", [1, D, fh], F32)
    p.I("w2_0", [1, fh, D], F32)
    p.I("w1_1", [NEXP // 2, D, FFN_EXP], F32)
    p.I("w3_1", [NEXP // 2, D, FFN_EXP], F32)
    p.I("w2_1", [NEXP // 2, FFN_EXP, D], F32)
    p.I("w_router_bc", [128, NEXP, D], F32)
    p.S("GQ", [9, 64, T], BF16)
    p.S("SMALL", [T, 9], F32)
    p.S("GATE", [T, 192], BF16)
    p.S("FV", [T, 192], BF16)
    p.S("FQ", [192, T], BF16)
    p.S("FK", [192, T], BF16)
    p.S("CU", [256, 30 + T], F32)
    p.S("DROW", [3, T], BF16)
    p.S("MA", [192, T], BF16)
    p.S("MB", [256, T], BF16)
    p.S("MC", [192, T], BF16)
    p.S("PART", [T, D], F32)
    p.S("SUM", [T, D], F32)
    p.S("XMID", [T, D], F32)
    p.S("XMT", [8, 128, T], BF16)
    p.S("GATES", [128, NT, NEXP], F32)
    p.S("X1", [T, D], F32)
    p.S("XT", [8, 128, T], BF16)
    p.O("OUT", [T, D], F32)
    nc = p.nc
    for l in range(2):
        d = dict(p.d)
        for n in MIX_PARAMS + TOK_PARAMS:
            d[n] = p.d[f"{n}_{l}"]
        moe = (l == 1)
        phase_A(nc, T, d, l == 0)
        phase_B(nc, T, d)
        phase_C(nc, T, d)
        phase_D(nc, T, d)
        phase_E1(nc, T, d, moe, "x" if l == 0 else "X1", groups)
        phase_E2(nc, T, d, moe, "")
        if l == 0:
            phase_E3(nc, T, d, "", "X1", "XT", groups)
        else:
            phase_E3(nc, T, d, "", "OUT", None, groups)
    return nc


def kernel(**inputs):
    inp = {k: np.asarray(v) for k, v in inputs.items()}
    B, T, _ = inp["x"].shape
    NC = 2 * B
    consts = make_consts()
    groups = [[2 * b, 2 * b + 1] for b in range(B)]
    nc = build_fused(T, groups)
    maps = [core_inputs(inp, c // 2, c % 2, consts) for c in range(NC)]
    res = run_bass_kernel_spmd(nc, maps, core_ids=list(range(NC))).results
    out = np.stack([np.asarray(res[2 * b]["OUT"]) for b in range(B)])
    return out.astype(np.float32)
```

```python
import contextlib
import math
import numpy as np
import ml_dtypes
import concourse.bass as bass
import concourse.mybir as mybir
from concourse.bass_utils import run_bass_kernel_spmd

F32 = mybir.dt.float32
BF16 = mybir.dt.bfloat16
ALU = mybir.AluOpType
AF = mybir.ActivationFunctionType
AX = mybir.AxisListType

ENGS = ["tensor", "vector", "scalar", "gpsimd", "sync"]

D = 1024
HD = 64
NGH = 3
NFH = 3
CONVW = 256
CK = 31
FFN_DENSE = 2816
NEXP = 8
FFN_EXP = 3584
ALPHA = 4 ** 0.25
LN_EPS = 1e-5
NORM_EPS = 1e-6
OFF = dict(a_q=0, a_k=384, a_v=768, a_decay=1152, a_beta=1158, a_gate=1164, glu_a=1548, glu_b=1804,
           f_q=2060, f_k=2444, f_v=2828, f_forget=3212)
BIG = 30000.0
DBG = set()
DBGV = {}
DQ2 = "gpsimd"


class Res:
    __slots__ = ("name", "last_w", "readers", "excl")

    def __init__(self, name, excl=False):
        self.name = name
        self.last_w = None
        self.readers = []
        self.excl = excl


class Op:
    __slots__ = ("eng", "fn", "deps", "is_dma", "signal", "count", "dma_sem", "dma_count", "is_cc")

    def __init__(self, eng, fn, is_dma, is_cc=False):
        self.eng = eng
        self.fn = fn
        self.deps = set()
        self.is_dma = is_dma or is_cc
        self.is_cc = is_cc
        self.signal = False
        self.count = 0
        self.dma_sem = None
        self.dma_count = 0


class Sched:
    NL = 8
    _uid = 0

    def __init__(self, nc):
        self.nc = nc
        self.ops = []

    def op(self, eng, fn, reads=(), writes=(), dma=False, cc=False):
        o = Op(eng, fn, dma, cc)
        idx = len(self.ops)
        writes = [r for r in writes if r is not None] + [r for r in reads if r is not None and r.excl]
        reads = [r for r in reads if r is not None and not r.excl]
        for r in reads:
            if r is not None and r.last_w is not None:
                o.deps.add(r.last_w)
        for r in writes:
            if r is None:
                continue
            if r.last_w is not None:
                o.deps.add(r.last_w)
            for rd in r.readers:
                o.deps.add(rd)
        for r in reads:
            if r is not None:
                r.readers.append(idx)
        for r in writes:
            if r is not None:
                r.last_w = idx
                r.readers = []
        o.deps.discard(idx)
        self.ops.append(o)
        return idx

    def emit(self):
        nc = self.nc
        ops = self.ops
        NL = self.NL
        for o in ops:
            if o.eng == "tensor" and not o.is_dma:
                o.deps = {d for d in o.deps if not (ops[d].eng == "tensor" and not ops[d].is_dma)}
        dn0 = {e: 0 for e in ENGS}
        for o in ops:
            if o.is_cc:
                o.dma_sem = ("cc", 0)
            elif o.is_dma:
                o.dma_sem = (o.eng, dn0[o.eng] % NL)
                dn0[o.eng] += 1
        for o in ops:
            best = {}
            for d in o.deps:
                od = ops[d]
                key = od.dma_sem if od.is_dma else od.eng
                if d > best.get(key, -1):
                    best[key] = d
            o.deps = set(best.values())
        for o in ops:
            for d in o.deps:
                ops[d].signal = True
        cnt = {e: 0 for e in ENGS}
        dn = {e: 0 for e in ENGS}
        lane_cnt = {}
        for o in ops:
            if o.is_cc:
                lane = ("cc", 0)
                lane_cnt[lane] = lane_cnt.get(lane, 0) + 1
                o.dma_sem = lane
                o.dma_count = lane_cnt[lane]
            elif o.is_dma:
                lane = (o.eng, dn[o.eng] % NL)
                dn[o.eng] += 1
                lane_cnt[lane] = lane_cnt.get(lane, 0) + 16
                o.dma_sem = lane
                o.dma_count = lane_cnt[lane]
            elif o.signal:
                cnt[o.eng] += 1
                o.count = cnt[o.eng]
        Sched._uid += 1
        u = Sched._uid
        sem = {e: nc.alloc_semaphore(name=f"s{u}_{e}") for e in ENGS}
        dsem = {}
        for lane in lane_cnt:
            dsem[lane] = nc.alloc_semaphore(name=f"d{u}_{lane[0]}_{lane[1]}")
        with contextlib.ExitStack() as st:
            block = st.enter_context(nc.Block())
            per_eng = {e: [] for e in ENGS}
            for i, o in enumerate(ops):
                per_eng[o.eng].append(i)

            def body_for(e):
                def body(engine):
                    waited = {}
                    for i in per_eng[e]:
                        o = ops[i]
                        need = {}
                        for d in o.deps:
                            od = ops[d]
                            key = ("d", od.dma_sem) if od.is_dma else ("c", od.eng)
                            v = od.dma_count if od.is_dma else od.count
                            if v > need.get(key, 0):
                                need[key] = v
                        if o.is_dma and not o.is_cc and o.dma_count > 16:
                            key = ("d", o.dma_sem)
                            need[key] = max(need.get(key, 0), o.dma_count - 16)
                        for key, v in need.items():
                            if waited.get(key, 0) >= v:
                                continue
                            s = dsem[key[1]] if key[0] == "d" else sem[key[1]]
                            engine.wait_ge(s, v)
                            waited[key] = v
                        ins = o.fn(engine)
                        if o.is_cc:
                            ins.then_inc(dsem[o.dma_sem])
                        elif o.is_dma:
                            ins.then_inc(dsem[o.dma_sem], 16)
                        elif o.signal:
                            ins.then_inc(sem[e], 1)
                    for lane, v in lane_cnt.items():
                        if lane[0] == e or (lane[0] == "cc" and e == "gpsimd"):
                            engine.wait_ge(dsem[lane], v)
                return body

            for e in ENGS:
                if per_eng[e]:
                    getattr(block, e)(body_for(e))
        nc.clear_and_free_semaphores(list(sem.values()) + list(dsem.values()))
        nc.all_engine_barrier()
        return cnt, dn


class V:
    __slots__ = ("ap", "r")

    def __init__(self, ap, r):
        self.ap = ap
        self.r = r

    def __getitem__(self, idx):
        return V(self.ap[idx], self.r)


class Tl:
    def __init__(self, t, r):
        self.t = t
        self.r = r

    def __getitem__(self, idx):
        return V(self.t[idx], self.r)


def _aps(x):
    return x.ap if isinstance(x, V) else x


def _rs(*xs):
    return [x.r for x in xs if isinstance(x, V) and x.r is not None]


class Phase:
    _uid = [0]

    def __init__(self, nc, name):
        self.nc = nc
        Phase._uid[0] += 1
        self.name = f"{name}{Phase._uid[0]}"

    def __enter__(self):
        self.st = contextlib.ExitStack()
        self.S = Sched(self.nc)
        self.n = 0
        return self

    def __exit__(self, *exc):
        if exc[0] is None:
            self.S.emit()
        self.st.close()
        return False

    def tile(self, shape, dtype, name=None):
        self.n += 1
        nm = f"{self.name}_{name or 't'}{self.n}"
        t = self.st.enter_context(self.nc.sbuf_tensor(nm, list(shape), dtype))
        return Tl(t, Res(nm))

    def psum(self, shape, dtype=F32, name=None):
        self.n += 1
        nm = f"{self.name}_{name or 'ps'}{self.n}"
        nb = 2 if dtype == BF16 else 4
        assert int(np.prod(shape[1:])) * nb == 2048 and shape[0] == 128, "PSUM tiles are whole banks"
        t = self.st.enter_context(self.nc.psum_tensor(nm, list(shape), dtype))
        return Tl(t, Res(nm, excl=True))

    def dma(self, q, out, in_, **kw):
        o, i = _aps(out), _aps(in_)
        return self.S.op(q, lambda e: e.dma_start(out=o, in_=i, **kw), reads=_rs(in_), writes=_rs(out), dma=True)

    def mm(self, out, lhsT, rhs, start=True, stop=True, extra_reads=()):
        o, l, r = _aps(out), _aps(lhsT), _aps(rhs)
        return self.S.op("tensor", lambda e: e.matmul(o, lhsT=l, rhs=r, start=start, stop=stop),
                         reads=_rs(lhsT, rhs) + list(extra_reads), writes=_rs(out))

    def tr(self, out, in_, ident):
        o, i, d = _aps(out), _aps(in_), _aps(ident)
        return self.S.op("tensor", lambda e: e.transpose(o, i, d), reads=_rs(in_, ident), writes=_rs(out))

    def act(self, out, in_, func, bias=None, scale=None, accum=None, eng="scalar"):
        o, i = _aps(out), _aps(in_)
        kw = {}
        if bias is not None:
            kw["bias"] = _aps(bias)
        if scale is not None:
            kw["scale"] = _aps(scale)
        if accum is not None:
            kw["accum_out"] = _aps(accum)
        return self.S.op("scalar", lambda e: e.activation(out=o, in_=i, func=func, **kw),
                         reads=_rs(in_, bias, scale), writes=_rs(out, accum))

    def ts(self, eng, out, in0, s1, s2, op0, op1=None, accum=None):
        o, i, a, b = _aps(out), _aps(in0), _aps(s1), _aps(s2)
        kw = {}
        if op1 is not None:
            kw["op1"] = op1
        if accum is not None:
            kw["accum_out"] = _aps(accum)
        return self.S.op(eng, lambda e: e.tensor_scalar(out=o, in0=i, scalar1=a, scalar2=b, op0=op0, **kw),
                         reads=_rs(in0, s1, s2), writes=_rs(out, accum))

    def tt(self, eng, out, in0, in1, op):
        o, i, j = _aps(out), _aps(in0), _aps(in1)
        return self.S.op(eng, lambda e: e.tensor_tensor(out=o, in0=i, in1=j, op=op),
                         reads=_rs(in0, in1), writes=_rs(out))

    def stt(self, eng, out, in0, scalar, in1, op0, op1, accum=None):
        o, i, s, j = _aps(out), _aps(in0), _aps(scalar), _aps(in1)
        kw = {}
        if accum is not None:
            kw["accum_out"] = _aps(accum)
        return self.S.op(eng, lambda e: e.scalar_tensor_tensor(out=o, in0=i, scalar=s, in1=j, op0=op0, op1=op1, **kw),
                         reads=_rs(in0, scalar, in1), writes=_rs(out, accum))

    def copy(self, eng, out, in_):
        o, i = _aps(out), _aps(in_)
        if eng == "scalar":
            return self.S.op(eng, lambda e: e.copy(out=o, in_=i), reads=_rs(in_), writes=_rs(out))
        return self.S.op(eng, lambda e: e.tensor_copy(out=o, in_=i), reads=_rs(in_), writes=_rs(out))

    def recip(self, out, in_, eng="vector"):
        o, i = _aps(out), _aps(in_)
        return self.S.op(eng, lambda e: e.reciprocal(out=o, in_=i), reads=_rs(in_), writes=_rs(out))

    def memset(self, eng, out, val):
        o = _aps(out)
        return self.S.op(eng, lambda e: e.memset(o, val), writes=_rs(out))

    def scan(self, out, d0, d1, init, op0, op1):
        o, a, b, c = _aps(out), _aps(d0), _aps(d1), _aps(init)
        return self.S.op("vector", lambda e: e.tensor_tensor_scan(out=o, data0=a, data1=b, initial=c, op0=op0, op1=op1),
                         reads=_rs(d0, d1, init), writes=_rs(out))


FM_GROUPS = [("g%s%d" % (t, h), 64) for h in range(3) for t in "qkv"] + [
             ("fq01", 128), ("fq2", 64), ("fk01", 128), ("fk2", 64),
             ("ca0", 128), ("cb0", 128), ("ca1", 128), ("cb1", 128)]
FM_OFF = {}
_o = 0
for _n, _m in FM_GROUPS:
    FM_OFF[_n] = (_o, _m)
    _o += _m
TM0 = _o
NTM = 9 + 192 + 192
NCOL = TM0 + NTM


def conv_perm(hh):
    return list(range(128 * hh, 128 * hh + 128)) + list(range(128 * (1 - hh), 128 * (1 - hh) + 128))


def pack_w_in(w_in_l, hh):
    def heads(base, h0, n):
        return list(range(base + (3 * hh + h0) * 64, base + (3 * hh + h0 + n) * 64))
    cols = []
    for h in range(3):
        cols += heads(OFF["a_q"], h, 1) + heads(OFF["a_k"], h, 1) + heads(OFF["a_v"], h, 1)
    cols += heads(OFF["f_q"], 0, 2) + heads(OFF["f_q"], 2, 1) + heads(OFF["f_k"], 0, 2) + heads(OFF["f_k"], 2, 1)
    perm = conv_perm(hh)
    cols += [OFF["glu_a"] + c for c in perm[:128]] + [OFF["glu_b"] + c for c in perm[:128]]
    cols += [OFF["glu_a"] + c for c in perm[128:]] + [OFF["glu_b"] + c for c in perm[128:]]
    cols += [OFF["a_decay"] + 3 * hh + i for i in range(3)] + [OFF["a_beta"] + 3 * hh + i for i in range(3)]
    cols += [OFF["f_forget"] + 3 * hh + i for i in range(3)]
    cols += heads(OFF["a_gate"], 0, 3) + heads(OFF["f_v"], 0, 3)
    assert len(cols) == NCOL
    return np.ascontiguousarray(w_in_l[:, cols])


def pack_gcw(gdn_conv_w_l, hh):
    out = np.zeros((128, 9, 4), np.float32)
    for h in range(3):
        for typ in range(3):
            ch0 = typ * 384 + (3 * hh + h) * 64
            out[:64, h * 3 + typ, :] = gdn_conv_w_l[:, ch0: ch0 + 64].T
    return out


def phase_A(nc, T, d, layer0):
    NB = T // 512
    with Phase(nc, "A") as P:
        ident = P.tile([128, 128], BF16, "ident")
        P.dma("sync", ident[:, :], d["ident_bf"])
        w = P.tile([128, 8, NCOL], BF16, "w")
        stage = [P.tile([128, NCOL], F32, "wst") for _ in range(2)]
        for k in range(8):
            s = stage[k % 2]
            P.dma("sync", s[:, :], d["w_in"][k * 128:(k + 1) * 128, :])
            P.copy(["vector", "gpsimd"][k % 2], w[:, k, :], s[:, :])
        gcw = P.tile([128, 9, 4], F32, "gcw")
        P.dma("sync", gcw[:, :, :], d["gcw"])
        bc9 = P.tile([128, 9], F32, "bc9")
        P.dma("sync", bc9[:, :], d["bc9"])
        dtb4 = P.tile([128, 4, 3], F32, "dtb4")
        negA4 = P.tile([128, 4, 3], F32, "negA4")
        fb4 = P.tile([128, 4, 3], F32, "fb4")
        eA = P.tile([128, 3], F32, "eA")
        P.act(eA[:, :], bc9[:, 3:6], AF.Exp)
        for tt in range(4):
            P.copy("vector", dtb4[:, tt, :], bc9[:, 0:3])
            P.ts("vector", negA4[:, tt, :], eA[:, :], -1.0, None, ALU.mult)
            P.copy("vector", fb4[:, tt, :], bc9[:, 6:9])

        fm_ps = [P.psum([128, 512], F32, "fm") for _ in range(4)]
        tm_ps = [P.psum([128, 512], F32, "tm") for _ in range(2)]
        xtb = [P.tile([128, 8, 512], BF16, "xt") for _ in range(2)]
        if layer0:
            tr_ps = [P.psum([128, 8, 128], BF16, "tr") for _ in range(2)]
            xst = [P.tile([128, D], F32, "xst") for _ in range(2)]
            xbf = [P.tile([128, D], BF16, "xbf") for _ in range(2)]
        raw = [P.tile([128, 515], F32, "raw") for _ in range(9)]
        for r in raw:
            P.memset("vector", r[:, 0:3], 0.0)
        acc = [P.tile([128, 512], F32, "acc") for _ in range(4)]
        ybf = [P.tile([128, 512], BF16, "ybf") for _ in range(4)]
        sig = [P.tile([128, 512], F32, "sig") for _ in range(2)]
        uu = [P.tile([128, 512], F32, "uu") for _ in range(2)]
        small = [P.tile([128, 4, 9], F32, "small") for _ in range(2)]
        sm_t = [P.tile([128, 4, 9], F32, "smt") for _ in range(2)]
        sm_o = [P.tile([128, 4, 9], F32, "smo") for _ in range(2)]
        gate_bf = [P.tile([128, 192], BF16, "gate") for _ in range(2)]
        fv_bf = [P.tile([128, 192], BF16, "fv") for _ in range(2)]
        zero = P.tile([128, 32], F32, "zero")
        P.memset("vector", zero[:, :], 0.0)
        for i in range(2):
            P.dma(DQ2, d["CU"][i * 128:(i + 1) * 128, 0:30], zero[:, 0:30])

        n_fm = 0
        n_y = 0
        n_a = 0
        n_tt = 0
        deferred = []
        for blk in range(NB):
            t0 = blk * 512
            xt = xtb[blk % 2]
            if layer0:
                for tt in range(4):
                    j = blk * 4 + tt
                    xs, xb, ps = xst[j % 2], xbf[j % 2], tr_ps[j % 2]
                    P.dma("sync", xs[:, :], d["x"][t0 + tt * 128: t0 + (tt + 1) * 128, :])
                    P.copy("gpsimd", xb[:, :], xs[:, :])
                    for k in range(8):
                        P.tr(ps[:, k, :], xb[:, k * 128:(k + 1) * 128], ident[:, :])
                    P.copy("scalar", xt[:, :, tt * 128:(tt + 1) * 128], ps[:, :, :])
            else:
                P.dma("sync", xt[:, :, :], d["XT"][:, :, t0:t0 + 512].rearrange("k p t -> p k t"))

            glu_a_ps = None
            for gi, (gname, M) in enumerate(FM_GROUPS):
                if ("gdn" in DBG and gi < 9) or ("fox" in DBG and gname[0] == "f") or ("glu" in DBG and gname[0] == "c"):
                    continue
                c0 = FM_OFF[gname][0]
                ps = fm_ps[n_fm % 4]
                n_fm += 1
                for k in range(8):
                    P.mm(ps[0:M, :], w[:, k, c0:c0 + M], xt[:, k, :], start=(k == 0), stop=(k == 7))
                if gi < 9:
                    r = raw[gi]
                    P.copy("scalar", r[0:M, 3:515], ps[0:M, :])
                    a = acc[n_a % 4]
                    n_a += 1
                    P.ts("vector", a[0:M, :], r[0:M, 0:512], gcw[0:M, gi, 0:1], None, ALU.mult)
                    for j in range(1, 4):
                        P.stt("vector", a[0:M, :], r[0:M, j:j + 512], gcw[0:M, gi, j:j + 1], a[0:M, :], ALU.mult, ALU.add)
                    P.copy("gpsimd", r[0:M, 0:3], r[0:M, 512:515])

                    def fin(a=a, gi=gi, M=M, t0=t0):
                        nonlocal n_y
                        y = ybf[n_y % 4]
                        n_y += 1
                        P.act(y[0:M, :], a[0:M, :], AF.Silu)
                        P.dma(DQ2, d["GQ"][gi, 0:M, t0:t0 + 512], y[0:M, :])
                    deferred.append(fin)
                    if len(deferred) > 2:
                        deferred.pop(0)()
                elif gname in ("fq01", "fq2", "fk01", "fk2"):
                    y = ybf[n_y % 4]
                    n_y += 1
                    if gname[1] == "q":
                        P.ts("vector", y[0:M, :], ps[0:M, :], 0.125, None, ALU.mult)
                    else:
                        P.copy("vector", y[0:M, :], ps[0:M, :])
                    dst = d["FQ"] if gname[1] == "q" else d["FK"]
                    r0 = 0 if gname.endswith("01") else 128
                    P.dma(DQ2, dst[r0:r0 + M, t0:t0 + 512], y[0:M, :])
                elif gname[1] == "a":
                    glu_a_ps = ps
                else:
                    i = int(gname[2])
                    sg, u = sig[i], uu[i]
                    P.act(sg[:, :], ps[:, :], AF.Sigmoid)
                    P.tt("vector", u[:, :], glu_a_ps[:, :], sg[:, :], ALU.mult)
                    P.dma(DQ2, d["CU"][i * 128:(i + 1) * 128, 30 + t0:30 + t0 + 512], u[:, :])

            while deferred:
                deferred.pop(0)()
            if "tm" in DBG:
                continue
            sm, st_, so = small[blk % 2], sm_t[blk % 2], sm_o[blk % 2]
            for tt in range(4):
                ps = tm_ps[n_tt % 2]
                g_bf, v_bf = gate_bf[n_tt % 2], fv_bf[n_tt % 2]
                n_tt += 1
                for k in range(8):
                    P.mm(ps[:, 0:NTM], xt[:, k, tt * 128:(tt + 1) * 128], w[:, k, TM0:TM0 + NTM],
                         start=(k == 0), stop=(k == 7))
                tok = slice(t0 + tt * 128, t0 + (tt + 1) * 128)
                if "nosm" not in DBG:
                    P.copy("vector", sm[:, tt, :], ps[:, 0:9])
                if "nogate" not in DBG:
                    P.act(g_bf[:, :], ps[:, 9:201], AF.Silu)
                    if "nogdma" not in DBG:
                        P.dma("sync", d["GATE"][tok, :], g_bf[:, :])
                if "nofv" not in DBG:
                    P.copy("vector", v_bf[:, :], ps[:, 201:393])
                    if "nofdma" not in DBG:
                        P.dma("sync", d["FV"][tok, :], v_bf[:, :])
            if "small" in DBG:
                continue
            P.tt("vector", st_[:, :, 0:3], sm[:, :, 0:3], dtb4[:, :, :], ALU.add)
            P.act(st_[:, :, 0:3], st_[:, :, 0:3], AF.Exp)
            P.act(st_[:, :, 0:3], st_[:, :, 0:3], AF.Ln, bias=1.0)
            P.tt("vector", so[:, :, 0:3], st_[:, :, 0:3], negA4[:, :, :], ALU.mult)
            P.act(st_[:, :, 3:6], sm[:, :, 3:6], AF.Exp, scale=-1.0)
            P.ts("vector", st_[:, :, 3:6], st_[:, :, 3:6], 1.0, None, ALU.add)
            P.recip(so[:, :, 3:6], st_[:, :, 3:6])
            P.tt("vector", st_[:, :, 6:9], sm[:, :, 6:9], fb4[:, :, :], ALU.add)
            P.act(st_[:, :, 6:9], st_[:, :, 6:9], AF.Exp, scale=-1.0)
            P.act(st_[:, :, 6:9], st_[:, :, 6:9], AF.Ln, bias=1.0)
            P.ts("vector", so[:, :, 6:9], st_[:, :, 6:9], -1.0, None, ALU.mult)
            P.dma("sync", d["SMALL"][t0:t0 + 512, :].rearrange("(t p) c -> p t c", p=128), so[:, :, :])


def pack_cpar(inp, l, hh):
    out = np.zeros((128, 2, 35), np.float32)
    perm = np.array(conv_perm(hh))
    for i in range(2):
        ch = perm[i * 128:(i + 1) * 128]
        out[:, i, 0:31] = inp["cnv_dw_w"][l][:, ch].T
        out[:, i, 31] = inp["cnv_dw_b"][l][ch]
        out[:, i, 32] = inp["cnv_ln_g"][l][ch]
        out[:, i, 33] = inp["cnv_ln_b"][l][ch]
        out[:, i, 34] = inp["mix_scale"][l][384 + ch]
    return out


def phase_C(nc, T, d):
    NB = T // 512
    with Phase(nc, "C") as P:
        ident = P.tile([128, 128], BF16, "ident")
        P.dma("sync", ident[:, :], d["ident_bf"])
        cpar = P.tile([128, 2, 35], F32, "cpar")
        P.dma("sync", cpar[:, :, :], d["cpar"])
        dg = P.tile([128, 2, CK, 128], BF16, "dg")
        for i in range(2):
            for k in range(CK):
                P.ts("vector", dg[:, i, k, :], ident[:, :], cpar[:, i, k:k + 1], None, ALU.mult)
        onesN = P.tile([128, 128], F32, "onesN")
        P.memset("vector", onesN[:, :], 1.0 / CONVW)
        epst = P.tile([128, 1], F32, "eps")
        P.memset("vector", epst[:, :], LN_EPS)
        cps = [P.psum([128, 512], F32, "cps") for _ in range(2)]
        mps = P.psum([128, 512], F32, "mps")
        qps = P.psum([128, 512], F32, "qps")
        uf = [P.tile([128, 542], F32, "uf") for _ in range(2)]
        ub = [P.tile([128, 542], BF16, "ub") for _ in range(2)]
        yt = [P.tile([128, 512], F32, "y") for _ in range(2)]
        ysq = [P.tile([128, 512], F32, "ysq") for _ in range(2)]
        mean = P.tile([128, 512], F32, "mean")
        t1 = P.tile([128, 512], F32, "t1")
        rstd = P.tile([128, 512], F32, "rstd")
        z = [P.tile([128, 512], F32, "z") for _ in range(2)]
        ob = [P.tile([128, 512], BF16, "ob") for _ in range(2)]
        for blk in range(NB):
            t0 = blk * 512
            for i in range(2):
                P.dma("sync", uf[i][:, :], d["CU"][i * 128:(i + 1) * 128, t0:t0 + 542])
                P.copy("gpsimd", ub[i][:, :], uf[i][:, :])
                for k in range(CK):
                    P.mm(cps[i][:, :], dg[:, i, k, :], ub[i][:, k:k + 512], start=(k == 0), stop=(k == CK - 1))
                P.act(yt[i][:, :], cps[i][:, :], AF.Identity, bias=cpar[:, i, 31:32])
                P.act(ysq[i][:, :], yt[i][:, :], AF.Square)
            for i in range(2):
                P.mm(mps[:, :], onesN[:, :], yt[i][:, :], start=(i == 0), stop=(i == 1))
            for i in range(2):
                P.mm(qps[:, :], onesN[:, :], ysq[i][:, :], start=(i == 0), stop=(i == 1))
            P.copy("scalar", mean[:, :], mps[:, :])
            P.stt("vector", t1[:, :], mean[:, :], -1.0, mean[:, :], ALU.mult, ALU.mult)
            P.tt("vector", t1[:, :], qps[:, :], t1[:, :], ALU.add)
            P.act(t1[:, :], t1[:, :], AF.Sqrt, bias=epst[:, 0:1])
            P.recip(rstd[:, :], t1[:, :])
            for i in range(2):
                P.tt("vector", z[i][:, :], yt[i][:, :], mean[:, :], ALU.subtract)
                P.tt("vector", z[i][:, :], z[i][:, :], rstd[:, :], ALU.mult)
                P.ts("vector", z[i][:, :], z[i][:, :], cpar[:, i, 32:33], cpar[:, i, 33:34], ALU.mult, ALU.add)
                P.act(z[i][:, :], z[i][:, :], AF.Silu)
                P.ts("vector", ob[i][:, :], z[i][:, :], cpar[:, i, 34:35], None, ALU.mult)
                P.dma(DQ2, d["MB"][i * 128:(i + 1) * 128, t0:t0 + 512], ob[i][:, :])


def make_consts():
    p = np.arange(128)
    c = {}
    c["ident_bf"] = np.eye(128, dtype=ml_dtypes.bfloat16)
    cf = np.zeros((128, 6, 128), np.float32)
    cf[:, 0, :] = (p[:, None] <= p[None, :])
    cf[:, 1, :] = 1.0
    cf[:, 2, :] = np.where(p[:, None] > p[None, :], -BIG, 0.0)
    cf[:, 3, :] = np.eye(128)
    cf[:, 4, :] = np.where(p[:, None] <= p[None, :], BIG, 0.0)
    cf[:, 5, :] = np.where(p[:, None] > p[None, :], -BIG, 0.0)
    c["cf32"] = cf
    sel = np.zeros((128, 64), np.float32)
    sel[64, :] = 1.0
    c["sel"] = sel
    return c


class DV(V):
    pass


def phase_D(nc, T, d):
    NBk = T // 128
    NI = T // 512
    with Phase(nc, "D") as P:
        cf = P.tile([128, 6, 128], F32, "cf")
        P.dma("sync", cf[:, :, :], d["cf32"])
        sel = P.tile([128, 64], F32, "sel")
        P.dma("sync", sel[:, :], d["sel"])
        fms = P.tile([64, 3], F32, "fms")
        P.dma("sync", fms[:, :], d["fms"])
        lf = P.tile([128, NBk, 3], F32, "lf")
        for j0 in range(0, NBk, 16):
            j1 = min(NBk, j0 + 16)
            P.dma("sync", lf[:, j0:j1, :], d["SMALL"][j0 * 128:j1 * 128, 6:9].rearrange("(j p) c -> p j c", p=128))
        lfh = P.tile([128, 3, NBk], F32, "lfh")
        for h in range(3):
            P.copy("vector", lfh[:, h, :], lf[:, :, h])
        setup_ps = P.psum([128, 512], F32, "sps")
        cw = P.tile([128, 3, NBk], F32, "cw")
        tot = P.tile([128, 3, NBk], F32, "tot")
        incl = P.tile([128, 3, NBk], F32, "incl")
        cc = P.tile([128, 3, NBk], F32, "cc")
        dl = P.tile([128, 3, NBk], F32, "dl")
        onesb = P.tile([128, NBk], F32, "onesb")
        P.memset("vector", onesb[:, :], 1.0)
        P.mm(setup_ps[:, 0:3 * NBk], cf[:, 0, :], lfh[:, :, :], start=True, stop=True)
        P.copy("vector", cw[:, :, :], setup_ps[:, 0:3 * NBk])
        P.mm(setup_ps[:, 0:3 * NBk], cf[:, 1, :], lfh[:, :, :], start=True, stop=True)
        P.copy("vector", tot[:, :, :], setup_ps[:, 0:3 * NBk])
        for h in range(3):
            P.scan(incl[:, h, :], onesb[:, :], tot[:, h, :], 0.0, ALU.mult, ALU.add)
        P.tt("vector", cc[:, :, :], cw[:, :, :], incl[:, :, :], ALU.add)
        P.tt("vector", cc[:, :, :], cc[:, :, :], tot[:, :, :], ALU.subtract)
        for h in range(3):
            for I in range(NI):
                P.ts("vector", dl[:, h, 4 * I:4 * I + 4], cc[:, h, 4 * I:4 * I + 4], incl[:, h, 4 * I + 3:4 * I + 4], None,
                     ALU.subtract)
        dlT = P.tile([128, 128], BF16, "dlT")
        drow = DV(d["DROW"], Res("drow"))
        for h in range(3):
            P.tr(setup_ps[0:NBk, 0:128], dl[:, h, :], cf[:, 3, :])
            P.copy("vector", dlT[0:NBk, :], setup_ps[0:NBk, 0:128])
            P.dma("sync", V(d["DROW"][h, :].rearrange("(j p) -> j p", p=128), drow.r), dlT[0:NBk, :])

        s_ps = [P.psum([128, 512], F32, "s") for _ in range(3)]
        o_ps = [P.psum([128, 512], F32, "o") for _ in range(3)]
        b_ps = P.psum([128, 512], F32, "b")
        pt = [[P.tile([128, 512], BF16, "pt") for _ in range(2)] for _ in range(3)]
        qa = [P.tile([65, T], BF16, "qa") for _ in range(3)]
        ka = [P.tile([65, T], BF16, "ka") for _ in range(3)]
        va = [P.tile([128, NBk, 65], BF16, "va") for _ in range(3)]
        bI = [[P.tile([128, NBk], F32, "bI") for _ in range(2)] for _ in range(3)]
        osb = [P.tile([65, 512], F32, "osb") for _ in range(3)]
        rec = [P.tile([64, 512], F32, "rec") for _ in range(3)]
        oo = [P.tile([64, 512], BF16, "oo") for _ in range(3)]
        for h in range(3):
            q, k, v = qa[h], ka[h], va[h]
            P.dma("sync", q[0:64, :], d["FQ"][h * 64:(h + 1) * 64, :])
            P.dma("sync", q[64:65, :], V(d["DROW"][h:h + 1, :], drow.r))
            P.dma("sync", k[0:64, :], d["FK"][h * 64:(h + 1) * 64, :])
            P.memset("vector", k[64:65, :], 1.0)
            for j0 in range(0, NBk, 16):
                j1 = min(NBk, j0 + 16)
                P.dma(DQ2, v[:, j0:j1, 0:64],
                      d["FV"][j0 * 128:j1 * 128, h * 64:(h + 1) * 64].rearrange("(j p) c -> p j c", p=128))
            P.memset("vector", v[:, :, 64:65], 1.0)

        def head(h):
            q, k, v = qa[h], ka[h], va[h]
            n_s = 0
            for I in range(NI):
                b = bI[h][I % 2]
                P.ts("vector", b[:, :], cc[:, h, :], -1.0, incl[:, h, 4 * I + 3:4 * I + 4], ALU.mult, ALU.add)
                ops_ = o_ps[h]
                jmax = 4 * I + 3
                for j in range(jmax + 1):
                    r = j - 4 * I
                    q0 = 128 * r if r > 0 else 0
                    sp = s_ps[h]
                    p_ = pt[h][n_s % 2]
                    n_s += 1
                    P.mm(sp[:, q0:512], k[0:65, 128 * j:128 * j + 128], q[0:65, 512 * I + q0:512 * I + 512],
                         start=True, stop=True)
                    yield
                    if r >= 0:
                        P.tt("vector", sp[:, q0:q0 + 128], sp[:, q0:q0 + 128], cf[:, 2, :], ALU.add)
                    P.act(p_[:, q0:512], sp[:, q0:512], AF.Exp, bias=b[:, j:j + 1])
                    yield
                    P.mm(ops_[0:65, q0:512], v[:, j, 0:65], p_[:, q0:512], start=(j == 0), stop=(j == jmax))
                ob_, rc, o_ = osb[h], rec[h], oo[h]
                P.copy("scalar", ob_[0:65, :], ops_[0:65, :])
                P.mm(b_ps[0:64, :], sel[0:65, :], ob_[0:65, :], start=True, stop=True)
                P.recip(rc[:, :], b_ps[0:64, :])
                P.stt("vector", o_[:, :], ob_[0:64, :], fms[:, h:h + 1], rc[:, :], ALU.mult, ALU.mult)
                P.dma(DQ2, d["MC"][h * 64:(h + 1) * 64, 512 * I:512 * I + 512], o_[:, :])
                yield

        gens = [head(h) for h in range(3)]
        for k_, g_ in enumerate(gens):
            for _ in range(k_):
                next(g_, None)
        live = list(gens)
        while live:
            for g_ in list(live):
                try:
                    next(g_)
                except StopIteration:
                    live.remove(g_)


def phase_B(nc, T, d):
    NCH = T // 128
    NMB = T // 512
    with Phase(nc, "B") as P:
        cf = P.tile([128, 6, 128], F32, "cf")
        P.dma("sync", cf[:, :, :], d["cf32"])
        U, ONES, IDF, PMASK, NMASK = cf[:, 0, :], cf[:, 1, :], cf[:, 3, :], cf[:, 4, :], cf[:, 5, :]
        ident = P.tile([128, 128], BF16, "ident")
        P.dma("sync", ident[:, :], d["ident_bf"])
        ones_bf = P.tile([128, 128], BF16, "ones_bf")
        P.memset("vector", ones_bf[:, :], 1.0)
        epsn = P.tile([128, 1], F32, "epsn")
        P.memset("vector", epsn[:, :], NORM_EPS)
        gnw = P.tile([128, 192], F32, "gnw")
        gms = P.tile([128, 192], F32, "gms")
        P.dma("sync", gnw[:, :], d["gnw"])
        P.dma("sync", gms[:, :], d["gms"])
        nwms = P.tile([128, 192], F32, "nwms")
        P.tt("vector", nwms[:, :], gnw[:, :], gms[:, :], ALU.mult)

        sm = P.tile([128, NCH, 6], F32, "sm")
        for j0 in range(0, NCH, 16):
            j1 = min(NCH, j0 + 16)
            P.dma("sync", sm[:, j0:j1, :], d["SMALL"][j0 * 128:j1 * 128, 0:6].rearrange("(j p) c -> p j c", p=128))
        gh = P.tile([128, 3, NCH], F32, "gh")
        bh = P.tile([128, 3, NCH], F32, "bh")
        for h in range(3):
            P.copy("vector", gh[:, h, :], sm[:, :, h])
            P.copy("vector", bh[:, h, :], sm[:, :, 3 + h])
        W = 3 * NCH
        set_ps = P.psum([128, 512], F32, "set")
        gc = P.tile([128, 3, NCH], F32, "gc")
        gtot = P.tile([128, 3, NCH], F32, "gtot")
        P.mm(set_ps[:, 0:W], U, gh[:, :, :])
        P.copy("vector", gc[:, :, :], set_ps[:, 0:W])
        P.mm(set_ps[:, 0:W], ONES, gh[:, :, :])
        P.copy("vector", gtot[:, :, :], set_ps[:, 0:W])
        negc = P.tile([128, 3, NCH], F32, "negc")
        negb = P.tile([128, 3, NCH], F32, "negb")
        bege = P.tile([128, 3, NCH], F32, "bege")
        kdsc = P.tile([128, 3, NCH], F32, "kdsc")
        egtot = P.tile([128, 3, NCH], F32, "egtot")
        P.ts("vector", negc[:, :, :], gc[:, :, :], -1.0, None, ALU.mult)
        P.ts("vector", negb[:, :, :], bh[:, :, :], -1.0, None, ALU.mult)
        P.act(bege[:, :, :], gc[:, :, :], AF.Exp)
        P.tt("vector", bege[:, :, :], bege[:, :, :], bh[:, :, :], ALU.mult)
        P.tt("vector", kdsc[:, :, :], gtot[:, :, :], gc[:, :, :], ALU.subtract)
        P.act(kdsc[:, :, :], kdsc[:, :, :], AF.Exp)
        P.act(egtot[:, :, :], gtot[:, :, :], AF.Exp)

        NL_ = 4
        bk = [[set_ps if (l_ == 0 and j_ == 0) else P.psum([128, 512], F32, f"bk{l_}{j_}") for j_ in range(2)]
              for l_ in range(NL_)]
        R2 = 6
        def rot(shape, dt, name, n=R2):
            return [P.tile(shape, dt, name) for _ in range(n)]
        qf, kf, vf = rot([64, 512], BF16, "qf", 6), rot([64, 512], BF16, "kf", 6), rot([64, 512], BF16, "vf", 6)
        sqt = rot([64, 512], BF16, "sqt", 2)
        rn = rot([64, 512], F32, "rn", 2)
        qn, kn = rot([64, 512], BF16, "qn", 6), rot([64, 512], BF16, "kn", 6)
        gate = rot([128, 192], BF16, "gate", 2)
        gw = rot([128, 192], F32, "gw", 2)
        G = rot([128, 128], F32, "G")
        dstr = rot([128, 128], F32, "dstr")
        decT = rot([128, 128], F32, "decT")
        egcb = rot([64, 128], F32, "egcb")
        Nt = [rot([128, 128], BF16, "N") for _ in range(2)]
        Mt = [rot([128, 128], BF16, "M") for _ in range(2)]
        Pf = rot([128, 128], F32, "Pf")
        Pb = rot([128, 128], BF16, "Pb")
        ktok, vtok = rot([128, 64], BF16, "ktok"), rot([128, 64], BF16, "vtok")
        vb, kbg, kd = rot([128, 64], BF16, "vb"), rot([128, 64], BF16, "kbg"), rot([128, 64], BF16, "kd")
        u_sb = rot([128, 64], F32, "u")
        wT = rot([64, 128], BF16, "wT")
        AT = rot([128, 128], BF16, "AT")
        qg = rot([64, 128], BF16, "qg")
        vnew = rot([128, 64], BF16, "vnew")
        Sf = [P.tile([64, 64], F32, "Sf") for _ in range(3)]
        Sb = [P.tile([64, 64], BF16, "Sb") for _ in range(3)]
        for h in range(3):
            P.memset("vector", Sf[h][:, :], 0.0)
            P.memset("vector", Sb[h][:, :], 0.0)
        junk = rot([128, 64], F32, "junk")
        ss = rot([128, 1], F32, "ss")
        rstd = rot([128, 1], F32, "rstd")
        on = rot([128, 64], BF16, "on")
        oT = [P.tile([64, 128], BF16, "oT") for _ in range(6)]

        u_i = 0
        WIN = NL_
        active = []
        n_units = [0]

        def rr_pass():
            for g_ in list(active):
                try:
                    next(g_)
                except StopIteration:
                    active.remove(g_)

        for mb in range(NMB):
            t0 = mb * 512
            for h in range(3):
                x = (mb % 2) * 3 + h
                q_, k_, v_ = qf[x], kf[x], vf[x]
                P.dma("sync", q_[:, :], d["GQ"][h * 3 + 0, :, t0:t0 + 512])
                P.dma("sync", k_[:, :], d["GQ"][h * 3 + 1, :, t0:t0 + 512])
                P.dma("sync", v_[:, :], d["GQ"][h * 3 + 2, :, t0:t0 + 512])
                for src, dst, scl in ((q_, qn[x], HD ** -0.5), (k_, kn[x], 1.0)):
                    s2, r_ = sqt[u_i % 2], rn[u_i % 2]
                    sp = bk[u_i % NL_][0]
                    u_i += 1
                    P.tt("vector", s2[:, :], src[:, :], src[:, :], ALU.mult)
                    P.mm(sp[0:64, :], ones_bf[0:64, 0:64], s2[:, :])
                    P.act(r_[:, :], sp[0:64, :], AF.Sqrt, bias=epsn[0:64, 0:1])
                    P.recip(r_[:, :], r_[:, :])
                    P.stt("vector", dst[:, :], src[:, :], scl, r_[:, :], ALU.mult, ALU.mult)
            for c in range(4):
                n = mb * 4 + c
                cs = slice(c * 128, (c + 1) * 128)
                gt, gw_ = gate[n % 2], gw[n % 2]
                while len(active) > 3:
                    rr_pass()
                P.dma(DQ2, gt[:, :], d["GATE"][n * 128:(n + 1) * 128, :])
                P.tt("gpsimd", gw_[:, :], gt[:, :], nwms[:, :], ALU.mult)
                def unit(h, lane, n=n, mb=mb, cs=cs, gw_=gw_):
                        b0, b1 = bk[lane]
                        x = (mb % 2) * 3 + h
                        y = h * 2 + n % 2
                        qn_, kn_, v_ = qn[x], kn[x], vf[x]
                        gcol = gh[:, h, n:n + 1]
                        P.ts("gpsimd", G[y][:, :], ONES, gcol, None, ALU.mult)
                        P.mm(b0[:, 0:128], G[y][:, :], U, start=True, stop=True)
                        P.mm(b0[:, 128:256], G[y][:, :], U, start=True, stop=False)
                        P.mm(b0[:, 128:256], IDF, PMASK, start=False, stop=True)
                        P.mm(b0[:, 256:384], G[y][:, :], U, start=True, stop=False)
                        P.mm(b0[:, 256:384], IDF, NMASK, start=False, stop=True)
                        P.act(egcb[y][:, :], b0[0:64, 0:128], AF.Exp)
                        P.act(dstr[y][:, :], b0[:, 128:256], AF.Exp, scale=-1.0, bias=gc[:, h, n:n + 1])
                        P.act(decT[y][:, :], b0[:, 256:384], AF.Exp, bias=negc[:, h, n:n + 1])
                        yield
                        P.mm(b1[:, 0:64], kn_[:, cs], ident[0:64, 0:64])
                        P.mm(b1[:, 64:128], v_[:, cs], ident[0:64, 0:64])
                        P.copy("vector", ktok[y][:, :], b1[:, 0:64])
                        P.copy("scalar", vtok[y][:, :], b1[:, 64:128])
                        P.ts("gpsimd", vb[y][:, :], vtok[y][:, :], bh[:, h, n:n + 1], None, ALU.mult)
                        P.ts("gpsimd", kbg[y][:, :], ktok[y][:, :], bege[:, h, n:n + 1], None, ALU.mult)
                        P.ts("gpsimd", kd[y][:, :], ktok[y][:, :], kdsc[:, h, n:n + 1], None, ALU.mult)
                        yield
                        P.mm(b0[:, 0:128], kn_[:, cs], kn_[:, cs])
                        P.mm(b0[:, 128:256], kn_[:, cs], qn_[:, cs])
                        N0, M0 = Nt[0][y], Mt[0][y]
                        P.stt("vector", N0[:, :], b0[:, 0:128], negb[:, h, n:n + 1], dstr[y][:, :], ALU.mult, ALU.mult)
                        P.tt("vector", AT[y][:, :], b0[:, 128:256], decT[y][:, :], ALU.mult)
                        P.mm(b1[:, 128:256], N0[:, :], ident[:, :])
                        P.copy("scalar", M0[:, :], b1[:, 128:256])
                        yield
                        P.tt("vector", Pf[y][:, :], M0[:, :], IDF, ALU.add)
                        P.copy("scalar", Pb[y][:, :], Pf[y][:, :])
                        cur = 0
                        for rnd in range(0, 7):
                            Nc, Mc = Nt[cur][y], Mt[cur][y]
                            Nn, Mn = Nt[1 - cur][y], Mt[1 - cur][y]
                            sp = bk[lane][rnd % 2]
                            pp = bk[lane][1 - rnd % 2]
                            if rnd < 6:
                                P.mm(sp[:, 0:128], Mc[:, :], Nc[:, :])
                                if rnd < 5:
                                    P.mm(sp[:, 128:256], Nc[:, :], Mc[:, :])
                            if rnd >= 1:
                                P.mm(pp[:, 256:384], Nc[:, :], Pb[y][:, :])
                            if rnd < 6:
                                P.copy("scalar", Nn[:, :], sp[:, 0:128])
                                if rnd < 5:
                                    P.copy("vector", Mn[:, :], sp[:, 128:256])
                            if rnd >= 1:
                                P.tt("vector", Pf[y][:, :], Pf[y][:, :], pp[:, 256:384], ALU.add)
                                P.copy("scalar", Pb[y][:, :], Pf[y][:, :])
                            cur = 1 - cur
                            yield
                        P.mm(b0[:, 256:320], Pb[y][:, :], vb[y][:, :])
                        P.mm(b0[0:64, 384:512], kbg[y][:, :], Pb[y][:, :])
                        P.copy("vector", u_sb[y][:, :], b0[:, 256:320])
                        P.copy("scalar", wT[y][:, :], b0[0:64, 384:512])
                        P.tt("gpsimd", qg[y][:, :], qn_[:, cs], egcb[y][:, :], ALU.mult)
                        yield
                        P.mm(b1[:, 0:64], wT[y][:, :], Sb[h][:, :])
                        P.tt("vector", vnew[y][:, :], u_sb[y][:, :], b1[:, 0:64], ALU.subtract)
                        P.mm(b1[:, 64:128], qg[y][:, :], Sb[h][:, :], start=True, stop=False)
                        P.mm(b1[:, 64:128], AT[y][:, :], vnew[y][:, :], start=False, stop=True)
                        P.mm(b1[0:64, 128:192], kd[y][:, :], vnew[y][:, :])
                        P.stt("vector", Sf[h][:, :], Sf[h][:, :], egtot[0:64, h, n:n + 1], b1[0:64, 128:192], ALU.mult, ALU.add)
                        P.copy("scalar", Sb[h][:, :], Sf[h][:, :])
                        P.act(junk[y][:, :], b1[:, 64:128], AF.Square, accum=ss[y][:, 0:1])
                        P.act(rstd[y][:, :], ss[y][:, :], AF.Sqrt, scale=1.0 / HD, bias=epsn[:, 0:1])
                        P.recip(rstd[y][:, :], rstd[y][:, :])
                        P.stt("vector", on[y][:, :], b1[:, 64:128], rstd[y][:, 0:1], gw_[:, h * 64:(h + 1) * 64], ALU.mult, ALU.mult)
                        P.mm(b0[0:64, 0:128], on[y][:, :], ident[:, :])
                        P.copy("scalar", oT[y][:, :], b0[0:64, 0:128])
                        P.dma(DQ2, d["MA"][h * 64:(h + 1) * 64, n * 128:(n + 1) * 128], oT[y][:, :])

                for h in range(3):
                    while len(active) >= WIN:
                        rr_pass()
                    active.append(unit(h, n_units[0] % NL_))
                    n_units[0] += 1
        while active:
            rr_pass()


def _layernorm_tile(P, y, g_bc, b_bc, out, st, mv, rstd, epst):
    P.S.op("vector", (lambda o, i: (lambda e: e.bn_stats(out=o, in_=i)))(st[:, 0:6].ap, y[:, 0:512].ap),
           reads=_rs(y[:, :]), writes=_rs(st[:, :]))
    P.S.op("vector", (lambda o, i: (lambda e: e.bn_stats(out=o, in_=i)))(st[:, 6:12].ap, y[:, 512:1024].ap),
           reads=_rs(y[:, :]), writes=_rs(st[:, :]))
    P.S.op("vector", (lambda o, i: (lambda e: e.bn_aggr(out=o, in_=i)))(mv[:, 0:2].ap, st[:, 0:12].ap),
           reads=_rs(st[:, :]), writes=_rs(mv[:, :]))
    P.act(rstd[:, :], mv[:, 1:2], AF.Sqrt, bias=epst[:, 0:1])
    P.recip(rstd[:, :], rstd[:, :])
    P.ts("vector", out[:, :], y[:, :], mv[:, 0:1], rstd[:, 0:1], ALU.subtract, ALU.mult)
    P.tt("gpsimd", out[:, :], out[:, :], g_bc[:, :], ALU.mult)
    P.tt("gpsimd", out[:, :], out[:, :], b_bc[:, :], ALU.add)


WO_PIECES = [("MA", 0, 128), ("MA", 128, 64), ("MC", 0, 128), ("MC", 128, 64), ("MB", 0, 128)]


def pack_w_out(w_out_l, hh):
    out = np.zeros((5, 128, D), np.float32)
    bases = {"MA": 192 * hh, "MC": 640 + 192 * hh, "MB": 384 + 128 * hh}
    for i, (nm, r0, n) in enumerate(WO_PIECES):
        out[i, :n] = w_out_l[bases[nm] + r0: bases[nm] + r0 + n]
    return out


def phase_E1(nc, T, d, moe, xres_name, groups):
    NT = T // 128
    with Phase(nc, "E1") as P:
        CH, src_res, dst_res = _cc_chunks(P, T, d, "PART", "SUM", groups)
        NCHK = len(src_res)
        TPC = CH // 128
        wo = P.tile([128, 5, D], BF16, "wo")
        stg = [P.tile([128, D], F32, "stg") for _ in range(2)]
        for i in range(5):
            P.dma("sync", stg[i % 2][:, :], d["w_out"][i])
            P.copy(["vector", "gpsimd"][i % 2], wo[:, i, :], stg[i % 2][:, :])
        m_ps = [P.psum([128, 512], F32, "m") for _ in range(4)]
        mixT = [P.tile([128, 5, 512], BF16, "mixT") for _ in range(2)]
        po = [P.tile([128, D], F32, "po") for _ in range(2)]
        ident = P.tile([128, 128], BF16, "ident")
        P.dma("sync", ident[:, :], d["ident_bf"])
        g_bc = P.tile([128, D], F32, "g_bc")
        b_bc = P.tile([128, D], F32, "b_bc")
        P.dma("sync", g_bc[:, :], d["lnm_g"])
        P.dma("sync", b_bc[:, :], d["lnm_b"])
        epst = P.tile([128, 1], F32, "eps")
        P.memset("vector", epst[:, :], LN_EPS)
        if moe:
            wr_bc = P.tile([128, NEXP, D], F32, "wr_bc")
            P.dma("sync", wr_bc[:, :, :], d["w_router_bc"])
            rjunk = P.tile([128, D], F32, "rjunk")
        t_ps = [P.psum([128, 8, 128], BF16, "t") for _ in range(2)]
        xres = [P.tile([128, D], F32, "xres") for _ in range(2)]
        msum = [P.tile([128, D], F32, "msum") for _ in range(2)]
        y = [P.tile([128, D], F32, "y") for _ in range(2)]
        xm = [P.tile([128, D], F32, "xm") for _ in range(2)]
        xmb = [P.tile([128, D], BF16, "xmb") for _ in range(2)]
        xmT = [P.tile([128, 8, 128], BF16, "xmT") for _ in range(2)]
        st = [P.tile([128, 12], F32, "st") for _ in range(2)]
        mv = [P.tile([128, 2], F32, "mv") for _ in range(2)]
        rstd = [P.tile([128, 1], F32, "rstd") for _ in range(2)]
        if moe:
            lg = [P.tile([128, NEXP], F32, "lg") for _ in range(2)]
            sc = [P.tile([128, 4 * NEXP + 8], F32, "sc") for _ in range(2)]
            gt = [P.tile([128, NEXP], F32, "gt") for _ in range(2)]

        def part_a(ci):
            for blk in range(ci * CH // 512, (ci + 1) * CH // 512):
                mt = mixT[blk % 2]
                for pi, (nm, r0, n) in enumerate(WO_PIECES):
                    P.dma("sync", mt[0:n, pi, :], d[nm][r0:r0 + n, blk * 512:(blk + 1) * 512])
                for tt in range(4):
                    i = blk * 4 + tt
                    a = i % 2
                    for half in range(2):
                        ps = m_ps[(2 * i + half) % 4]
                        hs = slice(half * 512, (half + 1) * 512)
                        for pi, (nm, r0, n) in enumerate(WO_PIECES):
                            P.mm(ps[:, :], mt[0:n, pi, tt * 128:(tt + 1) * 128], wo[0:n, pi, hs], start=(pi == 0), stop=(pi == 4))
                        P.copy(["vector", "scalar"][half], po[a][:, hs], ps[:, :])
                    P.dma("sync", V(d["PART"][i * 128:(i + 1) * 128, :], src_res[ci]), po[a][:, :])

        def part_b(ci):
            for i in range(ci * TPC, (ci + 1) * TPC):
                a = i % 2
                tok = slice(i * 128, (i + 1) * 128)
                P.dma("sync", xres[a][:, :], d[xres_name][tok, :])
                P.dma("sync", msum[a][:, :], V(d["SUM"][tok, :], dst_res[ci]))
                P.stt("vector", y[a][:, :], xres[a][:, :], ALPHA, msum[a][:, :], ALU.mult, ALU.add)
                _layernorm_tile(P, y[a], g_bc, b_bc, xm[a], st[a], mv[a], rstd[a], epst)
                P.dma("sync", d["XMID"][tok, :], xm[a][:, :])
                P.copy("gpsimd", xmb[a][:, :], xm[a][:, :])
                for k in range(8):
                    P.tr(t_ps[a][:, k, :], xmb[a][:, k * 128:(k + 1) * 128], ident[:, :])
                P.copy("scalar", xmT[a][:, :, :], t_ps[a][:, :, :])
                P.dma("sync", d["XMT"][:, :, tok].rearrange("k p t -> p k t"), xmT[a][:, :, :])
                if moe:
                    L, s_, g_ = lg[a], sc[a], gt[a]
                    for e_ in range(NEXP):
                        P.stt("vector", rjunk[:, :], xm[a][:, :], 1.0, wr_bc[:, e_, :], ALU.mult, ALU.mult,
                              accum=L[:, e_:e_ + 1])
                    m1, m2, nm1, dm = s_[:, 32:33], s_[:, 33:34], s_[:, 34:35], s_[:, 35:36]
                    eq1, l2, sel, ex = s_[:, 0:8], s_[:, 8:16], s_[:, 16:24], s_[:, 24:32]
                    P.S.op("vector", (lambda o, i_: (lambda e: e.reduce_max(out=o, in_=i_, axis=AX.X)))(m1.ap, L[:, :].ap),
                           reads=_rs(L[:, :]), writes=_rs(m1))
                    P.ts("vector", eq1, L[:, :], m1, None, ALU.is_equal)
                    P.stt("vector", l2, eq1, -BIG, L[:, :], ALU.mult, ALU.add)
                    P.S.op("vector", (lambda o, i_: (lambda e: e.reduce_max(out=o, in_=i_, axis=AX.X)))(m2.ap, l2.ap),
                           reads=_rs(l2), writes=_rs(m2))
                    P.ts("vector", sel, L[:, :], m2, None, ALU.is_ge)
                    P.ts("vector", nm1, m1, -1.0, None, ALU.mult)
                    P.act(ex, L[:, :], AF.Exp, bias=nm1)
                    P.tt("vector", dm, m2, m1, ALU.subtract)
                    P.act(dm, dm, AF.Exp)
                    P.ts("vector", dm, dm, 1.0, None, ALU.add)
                    P.recip(dm, dm)
                    P.tt("vector", g_[:, :], ex, sel, ALU.mult)
                    P.ts("vector", g_[:, :], g_[:, :], dm, None, ALU.mult)
                    P.dma("sync", d["GATES"][:, i, :], g_[:, :])

        LAG = 2
        for step in range(NCHK + LAG):
            if step < NCHK:
                part_a(step)
                _cc_issue(P, d, "PART", "SUM", groups, step * CH, min(CH, T - step * CH), src_res[step], dst_res[step])
            if step >= LAG:
                part_b(step - LAG)


def phase_E2(nc, T, d, moe, sfx, hh_experts=None):
    NT = T // 128
    NBK = T // 512
    NE = (NEXP // 2 if moe else 1) if 'ne' not in DBGV else DBGV['ne']
    F = FFN_EXP if moe else FFN_DENSE // 2
    NFH = 2 if moe else 1
    FHW = F // NFH
    FT = FHW // 128
    with Phase(nc, "E2") as P:
        w1h = P.tile([128, 8, FHW], BF16, "w1h")
        w3h = P.tile([128, 8, FHW], BF16, "w3h")
        w2h = P.tile([128, FT, D], BF16, "w2h")
        stg = [P.tile([128, 2048], F32, "stg") for _ in range(3)]
        gates = None
        if moe:
            gates = P.tile([128, NT, NEXP], F32, "gates")
            P.dma("sync", gates[:, :, :], d["GATES"])
        h_ps = [P.psum([128, 512], F32, "h") for _ in range(4)]
        y_ps = [P.psum([128, 512], F32, "y") for _ in range(2)]
        xmT = [P.tile([128, 8, 512], BF16, "xmT") for _ in range(2)]
        hT = P.tile([128, FT, 512], BF16, "hT")
        sg = [P.tile([128, 512], F32, "sg") for _ in range(2)]
        accL = [P.tile([128, D], F32, "accL") for _ in range(2)]
        accS = [P.tile([128, D], F32, "accS") for _ in range(2)]
        acc_res = [Res(f"acc{i}") for i in range(NT)]
        n_c = 0
        ceng = ["vector", "gpsimd", "scalar"]
        n_h = 0
        n_y = 0
        n_b = 0
        first = True
        for e in range(NE):
            for fh in range(NFH):
                f0 = fh * FHW
                for wsrc, wdst in ((d["w1" + sfx], w1h), (d["w3" + sfx], w3h)):
                    for k in range(8):
                        s = stg[n_c % 3]
                        P.dma("sync", s[:, 0:FHW], wsrc[e, k * 128:(k + 1) * 128, f0:f0 + FHW])
                        P.copy(ceng[n_c % 3], wdst[:, k, :], s[:, 0:FHW])
                        n_c += 1
                for ft in range(0, FT, 2):
                    nn = min(2, FT - ft)
                    s = stg[n_c % 3]
                    sv = V(s.t[:, 0:nn * D].rearrange("p (a c) -> p a c", a=nn), s.r)
                    P.dma("sync", sv, d["w2" + sfx][e, f0 + ft * 128:f0 + (ft + nn) * 128, :].rearrange("(a p) c -> p a c", p=128))
                    P.copy(ceng[n_c % 3], V(w2h.t[:, ft:ft + nn, :], w2h.r), sv)
                    n_c += 1
                for blk in range(NBK):
                    xt = xmT[n_b % 2]
                    n_b += 1
                    P.dma("sync", xt[:, :, :], d["XMT"][:, :, blk * 512:(blk + 1) * 512].rearrange("k p t -> p k t"))
                    for ft in range(FT):
                        p1, p3 = h_ps[n_h % 4], h_ps[(n_h + 1) % 4]
                        s_ = sg[(n_h // 2) % 2]
                        n_h += 2
                        fs = slice(ft * 128, (ft + 1) * 128)
                        for k in range(8):
                            P.mm(p1[:, :], w1h[:, k, fs], xt[:, k, :], start=(k == 0), stop=(k == 7))
                        for k in range(8):
                            P.mm(p3[:, :], w3h[:, k, fs], xt[:, k, :], start=(k == 0), stop=(k == 7))
                        P.act(s_[:, :], p1[:, :], AF.Silu)
                        P.tt("vector", hT[:, ft, :], s_[:, :], p3[:, :], ALU.mult)
                    for tt in range(4):
                        i = blk * 4 + tt
                        tok = slice(i * 128, (i + 1) * 128)
                        aL, aS = accL[i % 2], accS[i % 2]
                        accd = V(d["PART"][tok, :], acc_res[i])
                        if not first:
                            P.dma(DQ2, aL[:, :], accd)
                        for half in range(2):
                            yp = y_ps[n_y % 2]
                            n_y += 1
                            hs = slice(half * 512, (half + 1) * 512)
                            for ft in range(FT):
                                P.mm(yp[:, :], hT[:, ft, tt * 128:(tt + 1) * 128], w2h[:, ft, hs],
                                     start=(ft == 0), stop=(ft == FT - 1))
                            if first:
                                if moe:
                                    P.ts("vector", aS[:, hs], yp[:, :], gates[:, i, e:e + 1], None, ALU.mult)
                                else:
                                    P.copy("vector", aS[:, hs], yp[:, :])
                            else:
                                if moe:
                                    P.stt("vector", aS[:, hs], yp[:, :], gates[:, i, e:e + 1], aL[:, hs], ALU.mult, ALU.add)
                                else:
                                    P.tt("vector", aS[:, hs], yp[:, :], aL[:, hs], ALU.add)
                        P.dma(DQ2, accd, aS[:, :])
                first = False


def _cc_chunks(P, T, d, src, dst, groups):
    CH = 1024
    src_res, dst_res = [], []
    for i in range(0, T, CH):
        n = min(CH, T - i)
        src_res.append(Res(f"ccs{i}"))
        dst_res.append(Res(f"ccd{i}"))
    return CH, src_res, dst_res


def _cc_issue(P, d, src, dst, groups, i0, n, rs, rd):
    a, b = d[src][i0:i0 + n, :], d[dst][i0:i0 + n, :]
    P.S.op("gpsimd", (lambda a_, b_: (lambda e: e.collective_compute(
        "AllReduce", ALU.add, replica_groups=groups, ins=[a_.opt()], outs=[b_.opt()])))(a, b),
        reads=[rs], writes=[rd], cc=True)


def phase_E3(nc, T, d, sfx, out_name, xt_name, groups=None):
    NT = T // 128
    with Phase(nc, "E3") as P:
        CH, src_res, dst_res = _cc_chunks(P, T, d, "PART", "SUM", groups)
        if groups is not None:
            for ci in range(len(src_res)):
                _cc_issue(P, d, "PART", "SUM", groups, ci * CH, min(CH, T - ci * CH), src_res[ci], dst_res[ci])
        ident = P.tile([128, 128], BF16, "ident")
        P.dma("sync", ident[:, :], d["ident_bf"])
        g_bc = P.tile([128, D], F32, "g_bc")
        b_bc = P.tile([128, D], F32, "b_bc")
        P.dma("sync", g_bc[:, :], d["lnf_g" + sfx])
        P.dma("sync", b_bc[:, :], d["lnf_b" + sfx])
        epst = P.tile([128, 1], F32, "eps")
        P.memset("vector", epst[:, :], LN_EPS)
        t_ps = [P.psum([128, 8, 128], BF16, "t") for _ in range(2)]
        xm = [P.tile([128, D], F32, "xm") for _ in range(2)]
        ac = [P.tile([128, D], F32, "ac") for _ in range(2)]
        y = [P.tile([128, D], F32, "y") for _ in range(2)]
        o = [P.tile([128, D], F32, "o") for _ in range(2)]
        ob = [P.tile([128, D], BF16, "ob") for _ in range(2)]
        oT = [P.tile([128, 8, 128], BF16, "oT") for _ in range(2)]
        st = [P.tile([128, 12], F32, "st") for _ in range(2)]
        mv = [P.tile([128, 2], F32, "mv") for _ in range(2)]
        rstd = [P.tile([128, 1], F32, "rstd") for _ in range(2)]
        for i in range(NT):
            a = i % 2
            tok = slice(i * 128, (i + 1) * 128)
            P.dma("sync", xm[a][:, :], d["XMID"][tok, :])
            P.dma("sync", ac[a][:, :], V(d["SUM"][tok, :], dst_res[(i * 128) // CH]))
            P.stt("vector", y[a][:, :], xm[a][:, :], ALPHA, ac[a][:, :], ALU.mult, ALU.add)
            _layernorm_tile(P, y[a], g_bc, b_bc, o[a], st[a], mv[a], rstd[a], epst)
            P.dma(DQ2, d[out_name][tok, :], o[a][:, :])
            if xt_name is not None:
                P.copy("gpsimd", ob[a][:, :], o[a][:, :])
                for k in range(8):
                    P.tr(t_ps[a][:, k, :], ob[a][:, k * 128:(k + 1) * 128], ident[:, :])
                P.copy("scalar", oT[a][:, :, :], t_ps[a][:, :, :])
                P.dma(DQ2, d[xt_name][:, :, tok].rearrange("k p t -> p k t"), oT[a][:, :, :])


MIX_PARAMS = ["w_in", "gcw", "bc9", "fms", "cpar", "gnw", "gms"]
TOK_PARAMS = ["w_out", "lnm_g", "lnm_b", "lnf_g", "lnf_b", "w1", "w3", "w2"]


class Prog:
    def __init__(self):
        self.nc = bass.Bass("TRN2", target_bir_lowering=False)
        self.d = {}

    def I(self, n, shape, dt):
        self.d[n] = self.nc.dram_tensor(n, list(shape), dt, kind="ExternalInput").ap()

    def O(self, n, shape, dt):
        self.d[n] = self.nc.dram_tensor(n, list(shape), dt, kind="ExternalOutput").ap()

    def S(self, n, shape, dt):
        self.d[n] = self.nc.dram_tensor(n, list(shape), dt, kind="Internal").ap()


def expert_order(hh):
    return list(range(4 * hh, 4 * hh + 4)) + list(range(4 * (1 - hh), 4 * (1 - hh) + 4))


def core_inputs(inp, b, hh, consts):
    rep = lambda v: np.ascontiguousarray(np.tile(np.asarray(v, np.float32)[None], (128,) + (1,) * np.ndim(v)))
    m = dict(x=np.ascontiguousarray(inp["x"][b]), ident_bf=consts["ident_bf"], cf32=consts["cf32"], sel=consts["sel"])
    for l in range(2):
        ms = inp["mix_scale"][l]
        bc9 = np.concatenate([inp["gdn_dt_bias"][l, 3 * hh:3 * hh + 3], inp["gdn_a_log"][l, 3 * hh:3 * hh + 3],
                              inp["fox_f_bias"][l, 3 * hh:3 * hh + 3]]).astype(np.float32)
        m[f"w_in_{l}"] = pack_w_in(inp["w_in"][l], hh)
        m[f"gcw_{l}"] = pack_gcw(inp["gdn_conv_w"][l], hh)
        m[f"bc9_{l}"] = rep(bc9)
        m[f"fms_{l}"] = np.ascontiguousarray(ms[640 + 192 * hh:640 + 192 * hh + 192].reshape(3, 64).T)
        m[f"cpar_{l}"] = pack_cpar(inp, l, hh)
        m[f"gnw_{l}"] = rep(np.tile(inp["gdn_norm_w"][l], 3))
        m[f"gms_{l}"] = rep(ms[192 * hh:192 * hh + 192])
        m[f"w_out_{l}"] = pack_w_out(inp["w_out"][l], hh)
        m[f"lnm_g_{l}"] = rep(inp["ln_mix_g"][l])
        m[f"lnm_b_{l}"] = rep(inp["ln_mix_b"][l])
        m[f"lnf_g_{l}"] = rep(inp["ln_ffn_g"][l])
        m[f"lnf_b_{l}"] = rep(inp["ln_ffn_b"][l])
    fh = FFN_DENSE // 2
    m["w1_0"] = np.ascontiguousarray(inp["ffn_w1"][0:1, :, hh * fh:(hh + 1) * fh])
    m["w3_0"] = np.ascontiguousarray(inp["ffn_w3"][0:1, :, hh * fh:(hh + 1) * fh])
    m["w2_0"] = np.ascontiguousarray(inp["ffn_w2"][0:1, hh * fh:(hh + 1) * fh, :])
    es = slice(4 * hh, 4 * hh + 4)
    m["w1_1"] = np.ascontiguousarray(inp["moe_w1"][0, es])
    m["w3_1"] = np.ascontiguousarray(inp["moe_w3"][0, es])
    m["w2_1"] = np.ascontiguousarray(inp["moe_w2"][0, es])
    m["w_router_bc"] = rep(np.ascontiguousarray(inp["moe_router"][0].T[expert_order(hh)]))
    return m


def build_fused(T, groups):
    p = Prog()
    NT = T // 128
    fh = FFN_DENSE // 2
    p.I("x", [T, D], F32)
    p.I("ident_bf", [128, 128], BF16)
    p.I("cf32", [128, 6, 128], F32)
    p.I("sel", [128, 64], F32)
    for l in range(2):
        p.I(f"w_in_{l}", [D, NCOL], F32)
        p.I(f"gcw_{l}", [128, 9, 4], F32)
        p.I(f"bc9_{l}", [128, 9], F32)
        p.I(f"fms_{l}", [64, 3], F32)
        p.I(f"cpar_{l}", [128, 2, 35], F32)
        p.I(f"gnw_{l}", [128, 192], F32)
        p.I(f"gms_{l}", [128, 192], F32)
        p.I(f"w_out_{l}", [5, 128, D], F32)
        for n in ("lnm_g", "lnm_b", "lnf_g", "lnf_b"):
            p.I(f"{n}_{l}", [128, D], F32)
    p.I("w1_0", [1, D, fh], F32)
    p.I("w3_0", [1, D, fh], F32)
    p.I("w2_0", [1, fh, D], F32)
    p.I("w1_1", [NEXP // 2, D, FFN_EXP], F32)
    p.I("w3_1", [NEXP // 2, D, FFN_EXP], F32)
    p.I("w2_1", [NEXP // 2, FFN_EXP, D], F32)
    p.I("w_router_bc", [128, NEXP, D], F32)
    p.S("GQ", [9, 64, T], BF16)
    p.S("SMALL", [T, 9], F32)
    p.S("GATE", [T, 192], BF16)
    p.S("FV", [T, 192], BF16)
    p.S("FQ", [192, T], BF16)
    p.S("FK", [192, T], BF16)
    p.S("CU", [256, 30 + T], F32)
    p.S("DROW", [3, T], BF16)
    p.S("MA", [192, T], BF16)
    p.S("MB", [256, T], BF16)
    p.S("MC", [192, T], BF16)
    p.S("PART", [T, D], F32)
    p.S("SUM", [T, D], F32)
    p.S("XMID", [T, D], F32)
    p.S("XMT", [8, 128, T], BF16)
    p.S("GATES", [128, NT, NEXP], F32)
    p.S("X1", [T, D], F32)
    p.S("XT", [8, 128, T], BF16)
    p.O("OUT", [T, D], F32)
    nc = p.nc
    for l in range(2):
        d = dict(p.d)
        for n in MIX_PARAMS + TOK_PARAMS:
            d[n] = p.d[f"{n}_{l}"]
        moe = (l == 1)
        phase_A(nc, T, d, l == 0)
        phase_B(nc, T, d)
        phase_C(nc, T, d)
        phase_D(nc, T, d)
        phase_E1(nc, T, d, moe, "x" if l == 0 else "X1", groups)
        phase_E2(nc, T, d, moe, "")
        if l == 0:
            phase_E3(nc, T, d, "", "X1", "XT", groups)
        else:
            phase_E3(nc, T, d, "", "OUT", None, groups)
    return nc


def kernel(**inputs):
    inp = {k: np.asarray(v) for k, v in inputs.items()}
    B, T, _ = inp["x"].shape
    NC = 2 * B
    consts = make_consts()
    groups = [[2 * b, 2 * b + 1] for b in range(B)]
    nc = build_fused(T, groups)
    maps = [core_inputs(inp, c // 2, c % 2, consts) for c in range(NC)]
    res = run_bass_kernel_spmd(nc, maps, core_ids=list(range(NC))).results
    out = np.stack([np.asarray(res[2 * b]["OUT"]) for b in range(B)])
    return out.astype(np.float32)
```
